# Optimizing a Trainium2 kernel written in Bass

```python
import jax, jax.numpy as jnp
from jax import lax
import numpy as np

D_MODEL = 1024
BATCH = 4
SEQ = 4096
DEPTH = 1

CHUNK = 64

MIX_WIDTH = D_MODEL
CONV_WIDTH = MIX_WIDTH // 2
CONV_HEADS = 8
CONV_K = 3
POOL_WIDTH = MIX_WIDTH - CONV_WIDTH
POOL_WINDOWS = (2, 4, 8, 16)
POOL_GROUPS = len(POOL_WINDOWS)
POOL_GW = POOL_WIDTH // POOL_GROUPS
IN_PROJ = 3 * CONV_WIDTH + POOL_WIDTH

N_GROUPS = 4
EXPERTS_PER_GROUP = 8
TOP_K_EXPERTS = 2
D_EXPERT = D_MODEL // 4

PLE_DIM = 256
LN_EPS = 1e-5
DEEPNORM_ALPHA = (2.0 * DEPTH) ** 0.25
DEEPNORM_BETA = (8.0 * DEPTH) ** -0.25

kernel_name = "hybrid_conv_pool_hmoe_deepnorm_block"


def _layernorm(x, g, b):
    xf = x.astype(jnp.float32)
    mu = jnp.mean(xf, axis=-1, keepdims=True)
    var = jnp.mean(jnp.square(xf - mu), axis=-1, keepdims=True)
    y = (xf - mu) * lax.rsqrt(var + LN_EPS) * g.astype(jnp.float32) + b.astype(jnp.float32)
    return y.astype(x.dtype)


def _token_mixers(h, w_in, conv_w, conv_b, pool_w, pool_scale, w_out):
    bsz, seq, _ = h.shape
    u = h @ w_in
    b_g, c_g, v_c, v_p = jnp.split(u, [CONV_WIDTH, 2 * CONV_WIDTH, 3 * CONV_WIDTH], axis=-1)

    z = c_g * v_c
    zp = jnp.pad(z, ((0, 0), (CONV_K - 1, 0), (0, 0)))
    conv = sum(zp[:, k:k + seq] * conv_w[k] for k in range(CONV_K)) + conv_b
    y_conv = b_g * conv

    vp = v_p.reshape(bsz, seq, POOL_GROUPS, POOL_GW)
    cs0 = jnp.pad(jnp.cumsum(vp.astype(jnp.float32), axis=1), ((0, 0), (1, 0), (0, 0), (0, 0)))
    t = jnp.arange(seq)
    means = []
    for j, w in enumerate(POOL_WINDOWS):
        c = jnp.pad(cs0[:, :, j], ((0, 0), (w - 1, 0), (0, 0)))
        wsum = c[:, w:w + seq] - c[:, :seq]
        cnt = jnp.minimum(t + 1, w).astype(jnp.float32)[None, :, None]
        means.append(wsum / cnt)
    pooled = jnp.stack(means, axis=2).astype(h.dtype) - vp
    y_pool = jnp.einsum('bsgc,gcd->bsgd', pooled, pool_w).reshape(bsz, seq, POOL_WIDTH) * pool_scale

    return jnp.concatenate([y_conv, y_pool], axis=-1) @ w_out


def _hier_moe(h, w_rg, b_rg, w_re, b_re, w_gate, w_up, w_down):
    bsz, seq, d = h.shape
    tok = h.reshape(-1, d)
    g_logits = (tok @ w_rg).astype(jnp.float32) + b_rg.astype(jnp.float32)
    g_prob = jax.nn.softmax(g_logits, axis=-1)
    g_val, g_idx = lax.top_k(g_logits, 1)
    g_idx = g_idx[:, 0]
    g_w = jnp.take_along_axis(g_prob, g_idx[:, None], axis=-1)[:, 0]
    e_all = jnp.einsum('td,gde->tge', tok, w_re).astype(jnp.float32) + b_re.astype(jnp.float32)
    e_logits = jnp.take_along_axis(e_all, g_idx[:, None, None], axis=1)[:, 0]
    top_v, top_i = lax.top_k(e_logits, TOP_K_EXPERTS)
    top_w = jax.nn.softmax(top_v, axis=-1) * g_w[:, None]
    exp_w = jnp.sum(jax.nn.one_hot(top_i, EXPERTS_PER_GROUP, dtype=jnp.float32) * top_w[..., None], axis=1)
    combine = jax.nn.one_hot(g_idx, N_GROUPS, dtype=jnp.float32)[:, :, None] * exp_w[:, None, :]
    combine = combine.astype(h.dtype)
    out = jnp.zeros_like(tok)
    for g in range(N_GROUPS):
        hg = jnp.einsum('td,edf->tef', tok, w_gate[g])
        hu = jnp.einsum('td,edf->tef', tok, w_up[g])
        act = jax.nn.silu(hg) * hu * combine[:, g, :, None]
        out = out + jnp.einsum('tef,efd->td', act, w_down[g])
    return out.reshape(bsz, seq, d)


def setup_inputs(seed: int = 0) -> dict:
    key = jax.random.key(seed)
    ks = jax.random.split(key, 26)
    f32 = jnp.float32
    nrm = lambda k, shape, s: (jax.random.normal(k, shape, f32) * s)
    E = EXPERTS_PER_GROUP
    return {
        "x": nrm(ks[0], (BATCH, SEQ, D_MODEL), 1.0),
        "p": nrm(ks[1], (DEPTH, BATCH, SEQ, PLE_DIM), 1.0),
        "ln_in_g": 1.0 + nrm(ks[2], (D_MODEL,), 0.02),
        "ln_in_b": nrm(ks[3], (D_MODEL,), 0.02),
        "w_in": nrm(ks[4], (DEPTH, D_MODEL, IN_PROJ), D_MODEL ** -0.5),
        "conv_w": nrm(ks[5], (DEPTH, CONV_K, CONV_WIDTH), CONV_K ** -0.5),
        "conv_b": nrm(ks[6], (DEPTH, CONV_WIDTH), 0.02),
        "pool_w": nrm(ks[7], (DEPTH, POOL_GROUPS, POOL_GW, POOL_GW), POOL_GW ** -0.5),
        "pool_scale": 1.0 + nrm(ks[8], (DEPTH, POOL_WIDTH), 0.02),
        "w_out": nrm(ks[9], (DEPTH, MIX_WIDTH, D_MODEL), MIX_WIDTH ** -0.5 * DEEPNORM_BETA),
        "ln1_g": 1.0 + nrm(ks[10], (DEPTH, D_MODEL), 0.02),
        "ln1_b": nrm(ks[11], (DEPTH, D_MODEL), 0.02),
        "w_rg": nrm(ks[12], (DEPTH, D_MODEL, N_GROUPS), D_MODEL ** -0.5),
        "b_rg": nrm(ks[13], (DEPTH, N_GROUPS), 0.01),
        "w_re": nrm(ks[14], (DEPTH, N_GROUPS, D_MODEL, E), D_MODEL ** -0.5),
        "b_re": nrm(ks[15], (DEPTH, N_GROUPS, E), 0.01),
        "w_gate": nrm(ks[16], (DEPTH, N_GROUPS, E, D_MODEL, D_EXPERT), D_MODEL ** -0.5),
        "w_up": nrm(ks[17], (DEPTH, N_GROUPS, E, D_MODEL, D_EXPERT), D_MODEL ** -0.5),
        "w_down": nrm(ks[18], (DEPTH, N_GROUPS, E, D_EXPERT, D_MODEL), D_EXPERT ** -0.5 * DEEPNORM_BETA),
        "w_pg": nrm(ks[19], (DEPTH, D_MODEL, D_MODEL), D_MODEL ** -0.5),
        "b_pg": nrm(ks[20], (DEPTH, D_MODEL), 0.02),
        "w_ple": nrm(ks[21], (DEPTH, PLE_DIM, D_MODEL), PLE_DIM ** -0.5 * DEEPNORM_BETA),
        "ln2_g": 1.0 + nrm(ks[22], (DEPTH, D_MODEL), 0.02),
        "ln2_b": nrm(ks[23], (DEPTH, D_MODEL), 0.02),
    }


def reference(x, p, ln_in_g, ln_in_b, w_in, conv_w, conv_b, pool_w, pool_scale, w_out, ln1_g, ln1_b,
              w_rg, b_rg, w_re, b_re, w_gate, w_up, w_down, w_pg, b_pg, w_ple, ln2_g, ln2_b):
    h = _layernorm(x, ln_in_g, ln_in_b)
    for i in range(DEPTH):
        mix = _token_mixers(h, w_in[i], conv_w[i], conv_b[i], pool_w[i], pool_scale[i], w_out[i])
        h = _layernorm(DEEPNORM_ALPHA * h + mix, ln1_g[i], ln1_b[i])
        moe = _hier_moe(h, w_rg[i], b_rg[i], w_re[i], b_re[i], w_gate[i], w_up[i], w_down[i])
        ple = (p[i] @ w_ple[i]) * jax.nn.sigmoid(h @ w_pg[i] + b_pg[i])
        h = _layernorm(DEEPNORM_ALPHA * h + moe + ple, ln2_g[i], ln2_b[i])
    return h
```

```python
import numpy as np
import ml_dtypes
import concourse.bass as bass
import concourse.mybir as mybir
from concourse.bass_utils import run_bass_kernel_spmd

F32 = mybir.dt.float32
BF16 = mybir.dt.bfloat16
I32 = mybir.dt.int32
ALU = mybir.AluOpType
AF = mybir.ActivationFunctionType
AX = mybir.AxisListType

NCORES = 8
SCHED_A = ['A', 'A', 'B0', 'A', 'B0', 'B1', 'A', 'A', 'A', 'A', 'B1', 'A', 'B0', 'B1', 'A', 'B0', 'A', 'B0', 'B1', 'B1',
           'A']
_DBG = {}
CAP = 256
HPS = CAP // 128
NSLOT = 48
ZERO_INIT = False
D = 1024
TOK = 2048
HALO = 16
NT = TOK // 128
ST = 256
NST = TOK // ST
TPS = ST // 128
INP = 2048
NEXP = 32
DE = 256
PLE = 256
LN_EPS = 1e-5
ALPHA = 2.0 ** 0.25
POOL_W = (2, 4, 8, 16)
SB_BASE = 16512
SB_END = 229376


class Prog:
    ENG = ("pe", "act", "dve", "pool", "sp")

    def __init__(self):
        self.lists = {e: [] for e in self.ENG}
        self.count = {}
        self.waited = {e: {} for e in self.ENG}
        self.lastw = {}
        self.readers = {}

    def _waits(self, eng, r, w, is_dma):
        deps = []
        for n in list(r) + list(w):
            t = self.lastw.get(n)
            if t is not None:
                deps.append((t, True))
        for n in w:
            for t in self.readers.get(n, ()):
                deps.append((t, False))
        need = {}
        for (sk, v, src), is_w in deps:
            if src == eng and not is_dma:
                if eng == "pe":
                    continue
            if need.get(sk, 0) < v:
                need[sk] = v
        waits = []
        for sk, v in need.items():
            if self.waited[eng].get(sk, 0) < v:
                self.waited[eng][sk] = v
                waits.append((sk, v))
        return waits

    def _commit(self, tok, r, w):
        for n in w:
            self.lastw[n] = tok
            self.readers[n] = []
        for n in r:
            if n not in w:
                self.readers.setdefault(n, []).append(tok)

    def op(self, eng, fns, r=(), w=()):
        if not isinstance(fns, (list, tuple)):
            fns = [fns]
        waits = self._waits(eng, r, w, False)
        sk = "s_" + eng
        self.count[sk] = self.count.get(sk, 0) + 1
        tok = (sk, self.count[sk], eng)
        self.lists[eng].append((list(fns), waits, (sk, 1)))
        self._commit(tok, r, w)
        return tok

    def dma(self, q, fn, r=(), w=(), chan=None):
        waits = self._waits(q, r, w, True)
        if chan is None:
            chan = (list(w) + list(r))[0]
        sk = "d_" + chan
        self.count[sk] = self.count.get(sk, 0) + 16
        tok = (sk, self.count[sk], "dma")
        self.lists[q].append(([fn], waits, (sk, 16)))
        self._commit(tok, r, w)
        return tok

    def barrier(self):
        for e in self.ENG:
            waits = []
            for sk, v in self.count.items():
                if sk == "s_" + e and e == "pe":
                    continue
                if self.waited[e].get(sk, 0) < v:
                    self.waited[e][sk] = v
                    waits.append((sk, v))
            if waits:
                self.lists[e].append(([], waits, None))

    def final_wait(self, eng="sp"):
        waits = []
        for sk, v in self.count.items():
            if self.waited[eng].get(sk, 0) < v:
                self.waited[eng][sk] = v
                waits.append((sk, v))
        self.lists[eng].append(([], waits, None))

    def emit(self, eng, handle, sems):
        for fns, waits, inc in self.lists[eng]:
            for sk, v in waits:
                handle.wait_ge(sems[sk], v)
            ins = None
            for fn in fns:
                ins = fn(handle)
            if inc is not None:
                ins.then_inc(sems[inc[0]], inc[1])


class SbAlloc:
    def __init__(self, nc):
        self.nc = nc
        self.top = SB_BASE
        self.n = 0

    def mark(self):
        return self.top

    def reset(self, m):
        self.top = m

    def __call__(self, name, shape, dtype):
        esz = 4 if dtype in (F32, I32) else 2
        nbytes = esz
        for s in shape[1:]:
            nbytes *= s
        off = (self.top + 31) // 32 * 32
        assert off + nbytes <= SB_END, f"SBUF overflow at {name}: {off + nbytes}"
        self.top = off + nbytes
        self.n += 1
        return self.nc.alloc_sbuf_tensor_at(f"{name}_{self.n}", list(shape), dtype, offset=off)


def dram_ap(t, offset, pattern):
    return bass.AP(t, offset, [list(p) for p in pattern])


def build_nc(debug_out=None):
    nc = bass.Bass("TRN2", target_bir_lowering=False)
    P = Prog()
    sb = SbAlloc(nc)

    def din(name, shape, dt=F32):
        return nc.dram_tensor(name, list(shape), dt, kind="ExternalInput")

    x_d = din("x", [TOK + HALO, D])
    p_d = din("p", [TOK, PLE])
    hmask_d = din("hmask", [128, HALO])
    invcnt_d = din("invcnt", [128, 4 * HALO])
    ident_d = din("ident", [128, 128])
    vecs_d = din("vecs", [7, D])
    br_d = din("br", [36])
    w_in_d = din("w_in", [D, INP])
    w_out_d = din("w_out", [D, D])
    w_pg_d = din("w_pg", [D, D])
    w_ple_d = din("w_ple", [PLE, D])
    wr_d = din("wr", [D, 36])
    poolw_d = din("poolw", [128, 4 * 128])
    chv_d = din("chv", [128, 20])
    wgu0_d = din("wgu0", [NEXP, 128, 4 * 512])
    wgu1_d = din("wgu1", [NEXP, 128, 4 * 512])
    wdn_d = din("wdn", [NEXP, 128, 2 * 1024])
    tri_d = din("tri", [128, 128])
    cst_d = din("cst", [128, 179])
    zsrc_d = din("zsrc", [1024, D], BF16)
    out_d = nc.dram_tensor("out", [TOK, D], F32, kind="ExternalOutput")
    base_d = nc.dram_tensor("base_scr", [TOK, D], F32)
    xs_d = nc.dram_tensor("xs_scr", [NSLOT * CAP, D], BF16)
    ys_d = nc.dram_tensor("ys_scr", [NSLOT * CAP, D], BF16)

    psT = [nc.alloc_psum_tensor(f"psT{i}", [128, 1024], BF16) for i in range(2)]
    psF = nc.alloc_psum_tensor("psF", [128, 6 * 512], F32)
    bank_rr = [0]
    pair_rr = [0]

    pool_lo = [0]

    def bank():
        b = bank_rr[0] % 6
        bank_rr[0] += 1
        return f"psF{b}", psF[:, b * 512:(b + 1) * 512]

    def bank_a1():
        b = pool_lo[0] % 3
        pool_lo[0] += 1
        return f"psF{b}", psF[:, b * 512:(b + 1) * 512]

    def pair():
        b = pair_rr[0] % 3
        pair_rr[0] += 1
        return (f"psF{2 * b}", f"psF{2 * b + 1}"), psF[:, b * 1024:(b + 1) * 1024]

    ident = sb("ident", [128, 128], BF16)
    vbc = sb("vbc", [128, 4, D], F32)
    bpg_bf = sb("bpg_bf", [1, D], BF16)
    ones_row = sb("ones_row", [1, 128], BF16)
    br_bc = sb("br_bc", [128, 36], F32)
    chv = sb("chv", [128, 20], F32)
    hmask = sb("hmask", [128, HALO], F32)
    invcnt = sb("invcnt", [128, 4, HALO], F32)
    neghalf = sb("neghalf", [128, 1], F32)
    L_all = sb("L_all", [128, NT, 36], F32)
    h1b_all = sb("h1b_all", [128, NT, D], BF16)
    w12 = sb("w12", [128, 2, NT], F32)
    pos_i = sb("pos_i", [128, 2 * NT], I32)
    widx = sb("widx", [128, 64], I32)
    zt = sb("zt", [128, D], BF16) if ZERO_INIT else None
    NLN = 8
    lnst = [sb(f"lnst{j}", [128, 16], F32) for j in range(NLN)]
    ln_rr = [0]
    mark_persist = sb.mark()

    P.dma("pool", lambda e: e.dma_start(out=ident[:, :], in_=ident_d[:, :]), w=["ident"])
    for j in range(4):
        P.dma("sp", lambda e, j=j: e.dma_start(
            out=vbc[:, j, :], in_=dram_ap(vecs_d, j * D, [[0, 128], [1, D]])), w=[f"vbc{j}"])
    P.dma("pool", lambda e: e.dma_start(out=bpg_bf[0:1, :], in_=vecs_d[4:5, :]), w=["bpg_bf"])
    P.op("pool", lambda e: e.memset(ones_row[0:1, :], 1.0), w=["ones_row"])
    P.dma("sp", lambda e: e.dma_start(out=br_bc[:, :], in_=dram_ap(br_d, 0, [[0, 128], [1, 36]])), w=["br_bc"])
    P.dma("sp", lambda e: e.dma_start(out=chv[:, :], in_=chv_d[:, :]), w=["chv"])
    P.dma("sp", lambda e: e.dma_start(out=hmask[:, :], in_=hmask_d[:, :]), w=["hmask"])
    P.dma("sp", lambda e: e.dma_start(out=invcnt[:, :, :].rearrange("p g t -> p (g t)"), in_=invcnt_d[:, :]),
          w=["invcnt"])
    P.op("pool", lambda e: e.memset(neghalf[:, :], -0.5), w=["neghalf"])
    XZ = []
    if False:
        P.op("pool", lambda e: e.memset(zt[:, :], 0.0), w=["zt"])
        for j in range(NSLOT * HPS):
            P.dma("sp", lambda e, j=j: e.dma_start(out=xs_d[j * 128:(j + 1) * 128, :], in_=zt[:, :]),
                  r=["zt"], w=[f"xs_z{j}"], chan=f"xz{j % 4}")
            XZ.append(f"xs_z{j}")

    def layer_norm(src, src_n, tmp, tmp_n, dst, dst_n, gi, bi, np_=128, dstb=None, dstb_n=None, vb=None):
        vb = vbc if vb is None else vb
        j = ln_rr[0] % NLN
        ln_rr[0] += 1
        st = lnst[j]
        sn = f"lnst{j}"
        P.op("dve", lambda e: e.bn_stats(st[:np_, 0:6], src[:np_, 0:512]), r=[src_n], w=[sn + "a"])
        P.op("dve", lambda e: e.bn_stats(st[:np_, 6:12], src[:np_, 512:1024]), r=[src_n], w=[sn + "b"])
        P.op("dve", lambda e: e.bn_aggr(st[:np_, 12:14], st[:np_, 0:12]), r=[sn + "a", sn + "b"], w=[sn + "mv"])
        P.op("pool", lambda e: e.tensor_scalar(st[:np_, 14:15], st[:np_, 13:14], LN_EPS, None, ALU.add),
             r=[sn + "mv"], w=[sn + "ve"])
        P.op("pool", lambda e: e.tensor_tensor(st[:np_, 15:16], st[:np_, 14:15], neghalf[:np_, :], ALU.pow),
             r=[sn + "ve", "neghalf"], w=[sn + "rs"])
        P.op("dve", lambda e: e.scalar_tensor_tensor(tmp[:np_, :], src[:np_, :], st[:np_, 12:13],
                                                     vb[:np_, gi, :], ALU.subtract, ALU.mult),
             r=[src_n, sn + "mv", f"vbc{gi}"], w=[tmp_n])
        P.op("dve", lambda e: e.scalar_tensor_tensor(dst[:np_, :], tmp[:np_, :], st[:np_, 15:16],
                                                     vb[:np_, bi, :], ALU.mult, ALU.add),
             r=[tmp_n, sn + "rs", f"vbc{bi}"], w=[dst_n])
        if dstb is not None:
            P.op("act", lambda e: e.copy(dstb[:np_, :], dst[:np_, :]), r=[dst_n], w=[dstb_n])

    w_in_sb = sb("w_in_sb", [128, 8, INP], BF16)
    w_out_sb = sb("w_out_sb", [128, 8, D], BF16)
    w_pg_sb = sb("w_pg_sb", [128, 8, D], BF16)
    w_ple_sb = sb("w_ple_sb", [128, 2, D], BF16)
    wr_sb = sb("wr_sb", [128, 8, 36], BF16)
    poolw_sb = sb("poolw_sb", [128, 4, 128], BF16)
    zc = sb("zc", [128, 4, HALO], F32)
    vc = sb("vc", [128, 4, HALO], F32)
    xt = [sb(f"xt{i}", [128, D], F32) for i in range(2)]
    tmp1 = [sb(f"tmp1_{i}", [128, D], F32) for i in range(2)]
    tmp2 = sb("tmp2", [128, D], F32)
    tmp2a = sb("tmp2a", [128, D], F32)
    hbuf = [sb(f"h{i}", [128, D], F32) for i in range(4)]
    hb = [sb(f"hb{i}", [128, D], BF16) for i in range(2)]
    hT = [sb(f"hT{i}", [128, 8, ST], BF16) for i in range(2)]
    hTh = sb("hTh", [128, 8, HALO], BF16)
    cg = [sb(f"cg{i}", [128, ST], F32) for i in range(2)]
    z_ext = [sb(f"z_ext{i}", [128, HALO + ST], F32) for i in range(2)]
    acc = [sb(f"acc{i}", [128, ST], F32) for i in range(2)]
    vp_ext = [sb(f"vp_ext{i}", [128, HALO + ST], F32) for i in range(2)]
    tA = sb("tA", [128, HALO + ST], F32)
    tB = sb("tB", [128, HALO + ST], F32)
    tmpc = sb("tmpc", [128, HALO], F32)
    pooledT = [sb(f"pooledT{i}", [128, ST], BF16) for i in range(2)]
    ycatT = [sb(f"ycatT{i}", [128, 8, ST], BF16) for i in range(2)]
    h1T = [sb(f"h1T{i}", [128, 8, 128], BF16) for i in range(2)]
    pb = [sb(f"pb{i}", [128, PLE], BF16) for i in range(2)]
    pT = [sb(f"pT{i}", [128, 2, 128], BF16) for i in range(2)]

    for k in range(8):
        for hf in range(2):
            P.dma("pool", lambda e, k=k, hf=hf: e.dma_start(
                out=w_in_sb[:, k, hf * 1024:(hf + 1) * 1024],
                in_=w_in_d[k * 128:(k + 1) * 128, hf * 1024:(hf + 1) * 1024]), w=[f"w_in{k}_{hf}"], chan="w_in")
    P.dma("pool", lambda e: e.dma_start(out=poolw_sb[:, :, :].rearrange("p g d -> p (g d)"), in_=poolw_d[:, :]),
          w=["poolw"])
    def late_weights():
        for k in range(8):
            P.dma("pool", lambda e, k=k: e.dma_start(out=w_out_sb[:, k, :], in_=w_out_d[k * 128:(k + 1) * 128, :]),
                  w=[f"w_out{k}"], chan="w_out")
        for k in range(8):
            P.dma("pool", lambda e, k=k: e.dma_start(out=wr_sb[:, k, :], in_=wr_d[k * 128:(k + 1) * 128, :]),
                  w=[f"wr{k}"], chan="wr")
        for k in range(8):
            P.dma("pool", lambda e, k=k: e.dma_start(out=w_pg_sb[:, k, :], in_=w_pg_d[k * 128:(k + 1) * 128, :]),
                  w=[f"w_pg{k}"], chan="w_pg")
        for k in range(2):
            P.dma("pool", lambda e, k=k: e.dma_start(out=w_ple_sb[:, k, :], in_=w_ple_d[k * 128:(k + 1) * 128, :]),
                  w=[f"w_ple{k}"], chan="w_ple")

    W_IN = [f"w_in{k}_{hf}" for k in range(8) for hf in range(2)]
    W_OUT = [f"w_out{k}" for k in range(8)]
    W_R = [f"wr{k}" for k in range(8)]
    W_PG = [f"w_pg{k}" for k in range(8)]
    W_PLE = [f"w_ple{k}" for k in range(2)]

    def transposes(src, src_n, ps_i, np_, nchunk, csz=128):
        ps = psT[ps_i]
        fns = []
        for c in range(nchunk):
            fns.append(lambda e, c=c: e.transpose(ps[:csz, c * np_:(c + 1) * np_],
                                                  src[:np_, c * csz:(c + 1) * csz], ident[:np_, :np_]))
        P.op("pe", fns, r=[src_n, "ident"], w=[f"psT{ps_i}"])

    def halo_stage():
        P.dma("sp", lambda e: e.dma_start(out=xt[1][:HALO, :], in_=x_d[0:HALO, :]), w=["xt1"])
        layer_norm(xt[1], "xt1", tmp2, "tmp2", hbuf[3], "h3", 0, 1, np_=HALO, dstb=hb[1], dstb_n="hb1")
        transposes(hb[1], "hb1", 1, HALO, 8)
        P.op("act", lambda e: e.copy(hTh[:, :, :].rearrange("p k t -> p (k t)"), psT[1][:, 0:8 * HALO]),
             r=["psT1"], w=["hTh"])
        for q in range(4):
            bc_n, bc = bank()
            bv_n, bv = bank()
            for f, (bn, b) in ((4 + q, (bc_n, bc)), (8 + q, (bv_n, bv))):
                P.op("pe", [lambda e, k=k, f=f, b=b: e.matmul(b[:, 0:HALO], w_in_sb[:, k, f * 128:(f + 1) * 128],
                                                              hTh[:, k, :], start=(k == 0), stop=(k == 7))
                            for k in range(8)], r=["hTh"] + W_IN, w=[bn])
            P.op("act", lambda e, bc=bc: e.copy(cg[0][:, 0:HALO], bc[:, 0:HALO]), r=[bc_n], w=["cg0"])
            P.op("dve", lambda e, bv=bv: e.tensor_tensor(tmpc[:, :], bv[:, 0:HALO], cg[0][:, 0:HALO], ALU.mult),
                 r=[bv_n, "cg0"], w=["tmpc"])
            P.op("dve", lambda e, q=q: e.tensor_tensor(zc[:, q, :], tmpc[:, :], hmask[:, :], ALU.mult),
                 r=["tmpc", "hmask"], w=[f"zc{q}"])
        for g in range(4):
            bp_n, bp = bank()
            f = 12 + g
            P.op("pe", [lambda e, k=k, f=f, b=bp: e.matmul(b[:, 0:HALO], w_in_sb[:, k, f * 128:(f + 1) * 128],
                                                           hTh[:, k, :], start=(k == 0), stop=(k == 7))
                        for k in range(8)], r=["hTh"] + W_IN, w=[bp_n])
            P.op("dve", lambda e, g=g, bp=bp: e.tensor_tensor(vc[:, g, :], bp[:, 0:HALO], hmask[:, :], ALU.mult),
                 r=[bp_n, "hmask"], w=[f"vc{g}"])

    def x_prefetch(S):
        for m in range(TPS):
            i = S * TPS + m
            P.dma("sp", lambda e, i=i: e.dma_start(out=xt[i % 2][:, :],
                                                   in_=x_d[HALO + i * 128:HALO + (i + 1) * 128, :]), w=[f"xt{i % 2}"])

    def stage_a1(S):
        hs = S % 2
        for m in range(TPS):
            i = S * TPS + m
            xn = f"xt{i % 2}"
            layer_norm(xt[i % 2], xn, tmp2a, "tmp2a", hbuf[i % 4], f"h{i % 4}", 0, 1,
                       dstb=hb[i % 2], dstb_n=f"hb{i % 2}")
            if debug_out == "h":
                P.dma("sp", lambda e, i=i: e.dma_start(out=out_d[i * 128:(i + 1) * 128, :], in_=hbuf[i % 4][:, :]),
                      r=[f"h{i % 4}"], chan=f"st{i % 2}")
            yield
        if S + 1 < NST:
            x_prefetch(S + 1)
        if debug_out is None:
            for z in range(2 * S, min(2 * S + 2, (NSLOT * CAP) // 1024)):
                P.dma("sp", lambda e, z=z: e.dma_start(out=xs_d[z * 1024:(z + 1) * 1024, :], in_=zsrc_d[:, :]),
                      w=[f"xs_z{z}"], chan=f"xz{z % 2}")
                XZ.append(f"xs_z{z}")
        for m in range(TPS):
            i = S * TPS + m
            transposes(hb[i % 2], f"hb{i % 2}", i % 2, 128, 8)
            P.op("act", lambda e, i=i, m=m: e.copy(
                hT[hs][:, :, m * 128:(m + 1) * 128],
                psT[i % 2][:, :].rearrange("p (k t) -> p k t", k=8)), r=[f"psT{i % 2}"], w=[f"hT{hs}_{m}"])
        yield
        HTN = [f"hT{hs}_{m}" for m in range(TPS)]

        def inproj(f):
            bn, b = bank_a1()
            P.op("pe", [lambda e, k=k: e.matmul(b[:, 0:ST], w_in_sb[:, k, f * 128:(f + 1) * 128],
                                                hT[hs][:, k, :], start=(k == 0), stop=(k == 7))
                        for k in range(8)], r=HTN + W_IN, w=[bn])
            return bn, b

        def conv_chunk(q):
            zi = q % 2
            ze, zn, ac, an = z_ext[zi], f"z_ext{zi}", acc[zi], f"acc{zi}"
            bcn, bc_ = inproj(4 + q)
            bvn, bv_ = inproj(8 + q)
            P.op("act", lambda e, bc_=bc_: e.copy(cg[zi][:, :], bc_[:, 0:ST]), r=[bcn], w=[f"cg{zi}"])
            P.op("pool", lambda e, q=q, ze=ze: e.tensor_copy(ze[:, 0:HALO], zc[:, q, :]), r=[f"zc{q}"], w=[zn + "h"])
            P.op("dve", lambda e, bv_=bv_, ze=ze, zi=zi: e.tensor_tensor(ze[:, HALO:HALO + ST], bv_[:, 0:ST],
                                                                        cg[zi][:, :], ALU.mult),
                 r=[bvn, f"cg{zi}"], w=[zn + "b"])
            P.op("pool", lambda e, q=q, ze=ze: e.tensor_copy(zc[:, q, :], ze[:, ST:ST + HALO]),
                 r=[zn + "b"], w=[f"zc{q}"])
            P.op("pool", lambda e, q=q, ze=ze, ac=ac: e.tensor_scalar(
                ac[:, :], ze[:, HALO:HALO + ST], chv[:, 3 * q + 2:3 * q + 3], chv[:, 12 + q:13 + q],
                ALU.mult, ALU.add), r=[zn + "b", "chv"], w=[an])
            P.op("dve", lambda e, q=q, ze=ze, ac=ac: e.scalar_tensor_tensor(
                ac[:, :], ze[:, HALO - 1:HALO - 1 + ST], chv[:, 3 * q + 1:3 * q + 2], ac[:, :],
                ALU.mult, ALU.add), r=[zn + "b", zn + "h", "chv", an], w=[an])
            P.op("dve", lambda e, q=q, ze=ze, ac=ac: e.scalar_tensor_tensor(
                ac[:, :], ze[:, HALO - 2:HALO - 2 + ST], chv[:, 3 * q:3 * q + 1], ac[:, :],
                ALU.mult, ALU.add), r=[zn + "b", zn + "h", "chv", an], w=[an])
            bbn, bb_ = inproj(q)
            P.op("dve", lambda e, q=q, bb_=bb_, ac=ac: e.tensor_tensor(ycatT[hs][:, q, :], bb_[:, 0:ST], ac[:, :],
                                                                      ALU.mult),
                 r=[bbn, an], w=[f"ycatT{hs}_{q}"])

        def pool_chunk(g):
            vi = g % 2
            ve, vn = vp_ext[vi], f"vp_ext{vi}"
            bpn, bp_ = inproj(12 + g)
            P.op("pool", lambda e, g=g, ve=ve: e.tensor_copy(ve[:, 0:HALO], vc[:, g, :]), r=[f"vc{g}"], w=[vn + "h"])
            P.op("act", lambda e, bp_=bp_, ve=ve: e.copy(ve[:, HALO:HALO + ST], bp_[:, 0:ST]), r=[bpn], w=[vn + "b"])
            P.op("pool", lambda e, g=g, ve=ve: e.tensor_copy(vc[:, g, :], ve[:, ST:ST + HALO]),
                 r=[vn + "b"], w=[f"vc{g}"])
            E = HALO + ST
            P.op("pool", lambda e, ve=ve: e.tensor_tensor(tA[:, 1:E], ve[:, 1:E], ve[:, 0:E - 1], ALU.add),
                 r=[vn + "b", vn + "h"], w=["tA"])
            s_t, s_n = tA, "tA"
            if g >= 1:
                P.op("pool", lambda e: e.tensor_tensor(tB[:, 3:E], tA[:, 3:E], tA[:, 1:E - 2], ALU.add),
                     r=["tA"], w=["tB"])
                s_t, s_n = tB, "tB"
            if g >= 2:
                P.op("pool", lambda e: e.tensor_tensor(tA[:, 7:E], tB[:, 7:E], tB[:, 3:E - 4], ALU.add),
                     r=["tB"], w=["tA"])
                s_t, s_n = tA, "tA"
            if g >= 3:
                P.op("pool", lambda e: e.tensor_tensor(tB[:, 15:E], tA[:, 15:E], tA[:, 7:E - 8], ALU.add),
                     r=["tA"], w=["tB"])
                s_t, s_n = tB, "tB"
            pn = f"pooledT{vi}"
            P.op("dve", lambda e, g=g, ve=ve, s_t=s_t, vi=vi: e.scalar_tensor_tensor(
                pooledT[vi][:, :], s_t[:, HALO:E], 1.0 / POOL_W[g], ve[:, HALO:E], ALU.mult, ALU.subtract),
                r=[s_n, vn + "b"], w=[pn])
            if S == 0:
                P.op("dve", lambda e, g=g, s_t=s_t: e.tensor_tensor(tmpc[:, :], s_t[:, HALO:2 * HALO],
                                                                    invcnt[:, g, :], ALU.mult),
                     r=[s_n, "invcnt"], w=["tmpc"])
                P.op("dve", lambda e, ve=ve, vi=vi: e.tensor_tensor(pooledT[vi][:, 0:HALO], tmpc[:, :],
                                                                    ve[:, HALO:2 * HALO], ALU.subtract),
                     r=["tmpc", vn + "b", pn], w=[pn])
            bln, bl_ = bank_a1()
            P.op("pe", lambda e, g=g, vi=vi, bl_=bl_: e.matmul(bl_[:, 0:ST], poolw_sb[:, g, :], pooledT[vi][:, :],
                                                               start=True, stop=True),
                 r=[pn, "poolw"], w=[bln])
            P.op("act", lambda e, g=g, bl_=bl_: e.mul(ycatT[hs][:, 4 + g, :], bl_[:, 0:ST], chv[:, 16 + g:17 + g]),
                 r=[bln, "chv"], w=[f"ycatT{hs}_{4 + g}"])

        for q in range(4):
            conv_chunk(q)
            yield
            pool_chunk(q)
            yield

    def stage_a2(S, m):
        hs = S % 2
        YN = [f"ycatT{hs}_{c}" for c in range(8)]
        if True:
            i = S * TPS + m
            (pn0, pn1), pr = ("psF4", "psF5"), psF[:, 2048:3072]
            fns = []
            for hf in range(2):
                for k in range(8):
                    fns.append(lambda e, hf=hf, k=k: e.matmul(
                        pr[:, hf * 512:(hf + 1) * 512], ycatT[hs][:, k, m * 128:(m + 1) * 128],
                        w_out_sb[:, k, hf * 512:(hf + 1) * 512], start=(k == 0), stop=(k == 7)))
            P.op("pe", fns, r=YN + W_OUT, w=[pn0, pn1])
            t1, t1n = tmp1[i % 2], f"tmp1_{i % 2}"
            P.op("dve", lambda e, i=i, pr=pr, t1=t1: e.scalar_tensor_tensor(
                t1[:, :], hbuf[i % 4][:, :], ALPHA, pr[:, :], ALU.mult, ALU.add),
                r=[f"h{i % 4}", pn0, pn1], w=[t1n])
            yield
            layer_norm(t1, t1n, tmp2, "tmp2", t1, t1n, 2, 3, dstb=h1b_all[:, i, :], dstb_n=f"h1b{i}")
            yield
            transposes(h1b_all[:, i, :], f"h1b{i}", i % 2, 128, 8)
            P.op("act", lambda e, i=i: e.copy(
                h1T[i % 2][:, :, :],
                psT[i % 2][:, :].rearrange("p (k t) -> p k t", k=8)), r=[f"psT{i % 2}"], w=[f"h1T{i % 2}"])
            yield
            yield from stage_a3(i)

    def stage_a3(i):
        t1, t1n = tmp1[i % 2], f"tmp1_{i % 2}"
        if debug_out == "h1":
            P.dma("sp", lambda e: e.dma_start(out=out_d[i * 128:(i + 1) * 128, :], in_=t1[:, :]), r=[t1n],
                  chan=f"st{i % 2}")
        bn, b = "psF3", psF[:, 3 * 512:4 * 512]
        P.op("pe", [lambda e, k=k: e.matmul(b[:, 0:36], h1T[i % 2][:, k, :], wr_sb[:, k, :],
                                            start=(k == 0), stop=(k == 7)) for k in range(8)],
             r=[f"h1T{i % 2}"] + W_R, w=[bn])
        P.op("dve", lambda e: e.tensor_tensor(L_all[:, i, :], b[:, 0:36], br_bc[:, :], ALU.add),
             r=[bn, "br_bc"], w=[f"L{i}"])
        pi = i % 2
        P.dma("pool", lambda e: e.dma_start(out=pb[pi][:, :], in_=p_d[i * 128:(i + 1) * 128, :]), w=[f"pb{pi}"])
        transposes(pb[pi], f"pb{pi}", pi, 128, 2)
        P.op("act", lambda e: e.copy(pT[pi][:, :, :].rearrange("p c t -> p (c t)"), psT[pi][:, 0:256]),
             r=[f"psT{pi}"], w=[f"pT{pi}"])
        for hf in range(2):
            cs = slice(hf * 512, (hf + 1) * 512)
            gn, pg_ = "psF4", psF[:, 4 * 512:5 * 512]
            an, pa = "psF5", psF[:, 5 * 512:6 * 512]
            P.op("pe", [lambda e, k=k, cs=cs, pg_=pg_: e.matmul(pg_[:, :], h1T[i % 2][:, k, :], w_pg_sb[:, k, cs],
                                                               start=(k == 0), stop=False) for k in range(8)]
                 + [lambda e, cs=cs, pg_=pg_: e.matmul(pg_[:, :], ones_row[0:1, :], bpg_bf[0:1, cs],
                                                      start=False, stop=True)],
                 r=[f"h1T{i % 2}", "ones_row", "bpg_bf"] + W_PG, w=[gn])
            P.op("pe", [lambda e, c=c, cs=cs, pa=pa: e.matmul(pa[:, :], pT[pi][:, c, :], w_ple_sb[:, c, cs],
                                                             start=(c == 0), stop=(c == 1)) for c in range(2)],
                 r=[f"pT{pi}"] + W_PLE, w=[an])
            tn = f"tmp2_{hf}"
            P.op("act", lambda e, cs=cs, pg_=pg_: e.activation(tmp2[:, cs], pg_[:, :], AF.Sigmoid),
                 r=[gn], w=[tn, "tmp2"])
            P.op("dve", lambda e, cs=cs, pa=pa: e.tensor_tensor(tmp2[:, cs], pa[:, :], tmp2[:, cs], ALU.mult),
                 r=[an, tn], w=[tn])
            if hf == 0:
                yield
        P.op("dve", lambda e: e.scalar_tensor_tensor(t1[:, :], t1[:, :], ALPHA, tmp2[:, :], ALU.mult, ALU.add),
             r=[t1n, "tmp2_0", "tmp2_1"], w=[t1n, "tmp2"])
        if debug_out is None:
            P.dma("sp", lambda e: e.dma_start(out=base_d[i * 128:(i + 1) * 128, :], in_=t1[:, :]), r=[t1n],
                  w=[f"base_d{i}"], chan=f"st{i % 2}")
        yield

    def run_sched(gens, sched):
        alive = dict(gens)
        for key in sched:
            g = alive.get(key)
            if g is None:
                continue
            try:
                next(g)
            except StopIteration:
                del alive[key]
        run_rr(list(alive.values()))

    def run_rr(gens):
        gens = list(gens)
        while gens:
            for g in list(gens):
                try:
                    next(g)
                except StopIteration:
                    gens.remove(g)

    halo_stage()
    x_prefetch(0)
    run_rr([stage_a1(0)])
    late_weights()
    SCHED = list(SCHED_A)
    for S in range(NST):
        gens = {"B0": stage_a2(S, 0), "B1": stage_a2(S, 1)}
        if S + 1 < NST:
            gens["A"] = stage_a1(S + 1)
        run_sched(gens, SCHED)

    if debug_out is None:
        P.barrier()
        sb.reset(mark_persist)
        rt = {}

        def T(name, shape, dt=F32):
            rt[name] = sb("rt_" + name, shape, dt)
            return rt[name]

        LN_ = [f"L{i}" for i in range(NT)]
        tri = T("tri", [128, 128], BF16)
        ones = T("ones", [128, 128], BF16)
        cst = T("cst", [128, 179])
        P.dma("pool", lambda e: e.dma_start(out=tri[:, :], in_=tri_d[:, :]), w=["tri"])
        P.dma("sp", lambda e: e.dma_start(out=cst[:, :], in_=cst_d[:, :]), w=["cst"])
        P.op("pool", lambda e: e.memset(ones[:, :], 1.0), w=["ones"])
        gmax = T("gmax", [128, NT])
        ohg = T("ohg", [128, NT, 4])
        ex = T("ex", [128, NT, 4])
        se = T("se", [128, NT])
        gw = T("gw", [128, NT])
        el = T("el", [128, NT, 8])
        elt = T("elt", [128, NT, 8])
        m1 = T("m1", [128, NT])
        oh1 = T("oh1", [128, NT, 8])
        el2 = T("el2", [128, NT, 8])
        m2 = T("m2", [128, NT])
        oh2 = T("oh2", [128, NT, 8])
        dd = T("dd", [128, NT])
        A1 = T("A1", [128, NT, 32])
        A2 = T("A2", [128, NT, 32])
        Ab = T("Ab", [128, NT, 32], BF16)
        pre = T("pre", [128, NT + 1, 32])
        cmpa = T("cmpa", [128, 32, 16])
        ns = T("ns", [128, 32])
        scA = T("scA", [128, 32])
        scB = T("scB", [128, 32])
        sbase = T("sbase", [128, 32])
        R = T("R", [128, NT, 32])
        prod = T("prod", [128, NT, 32])
        posf = T("posf", [128, NT, 2])
        cmpb = T("cmpb", [128, 64, 32])
        esf = T("esf", [128, 64])

        def bc(t, n):
            return t[:, :].unsqueeze(2).to_broadcast([128, NT, n])

        gl = L_all[:, :, 0:4]
        P.op("dve", lambda e: e.reduce_max(gmax[:, :], gl, AX.X), r=LN_, w=["gmax"])
        P.op("dve", lambda e: e.tensor_tensor(ohg[:, :, :], gl, bc(gmax, 4), ALU.is_equal), r=LN_ + ["gmax"], w=["ohg"])
        P.op("dve", lambda e: e.tensor_tensor(ex[:, :, :], gl, bc(gmax, 4), ALU.subtract), r=LN_ + ["gmax"], w=["ex"])
        P.op("act", lambda e: e.activation(ex[:, :, :], ex[:, :, :], AF.Exp), r=["ex"], w=["ex"])
        P.op("dve", lambda e: e.reduce_sum(se[:, :], ex[:, :, :], AX.X), r=["ex"], w=["se"])
        P.op("dve", lambda e: e.reciprocal(gw[:, :], se[:, :]), r=["se"], w=["gw"])
        for g in range(4):
            dst = el if g == 0 else elt
            dn = "el" if g == 0 else "elt"
            P.op("dve", lambda e, g=g, dst=dst: e.tensor_tensor(
                dst[:, :, :], L_all[:, :, 4 + 8 * g:12 + 8 * g],
                ohg[:, :, g:g + 1].to_broadcast([128, NT, 8]), ALU.mult), r=LN_ + ["ohg"], w=[dn])
            if g > 0:
                P.op("dve", lambda e: e.tensor_tensor(el[:, :, :], el[:, :, :], elt[:, :, :], ALU.add),
                     r=["el", "elt"], w=["el"])
        P.op("dve", lambda e: e.reduce_max(m1[:, :], el[:, :, :], AX.X), r=["el"], w=["m1"])
        P.op("dve", lambda e: e.tensor_tensor(oh1[:, :, :], el[:, :, :], bc(m1, 8), ALU.is_equal),
             r=["el", "m1"], w=["oh1"])
        P.op("dve", lambda e: e.scalar_tensor_tensor(
            el2[:, :, :].rearrange("p a b -> p (a b)"), oh1[:, :, :].rearrange("p a b -> p (a b)"), -1e30,
            el[:, :, :].rearrange("p a b -> p (a b)"), ALU.mult, ALU.add), r=["oh1", "el"], w=["el2"])
        P.op("dve", lambda e: e.reduce_max(m2[:, :], el2[:, :, :], AX.X), r=["el2"], w=["m2"])
        P.op("dve", lambda e: e.tensor_tensor(oh2[:, :, :], el2[:, :, :], bc(m2, 8), ALU.is_equal),
             r=["el2", "m2"], w=["oh2"])
        P.op("dve", lambda e: e.tensor_tensor(dd[:, :], m2[:, :], m1[:, :], ALU.subtract), r=["m1", "m2"], w=["dd"])
        P.op("act", lambda e: e.activation(dd[:, :], dd[:, :], AF.Exp), r=["dd"], w=["dd"])
        w1 = w12[:, 0, :]
        w2 = w12[:, 1, :]
        P.op("dve", lambda e: e.tensor_scalar(w1, dd[:, :], 1.0, None, ALU.add), r=["dd"], w=["w1"])
        P.op("dve", lambda e: e.reciprocal(w1, w1), r=["w1"], w=["w1"])
        P.op("dve", lambda e: e.tensor_tensor(w1, w1, gw[:, :], ALU.mult), r=["w1", "gw"], w=["w1"])
        P.op("dve", lambda e: e.tensor_tensor(w2, dd[:, :], w1, ALU.mult), r=["dd", "w1"], w=["w2"])
        for g in range(4):
            P.op("dve", lambda e, g=g: e.tensor_tensor(
                A1[:, :, 8 * g:8 * g + 8], oh1[:, :, :], ohg[:, :, g:g + 1].to_broadcast([128, NT, 8]), ALU.mult),
                r=["oh1", "ohg"], w=[f"A1_{g}"])
            P.op("dve", lambda e, g=g: e.tensor_tensor(
                A2[:, :, 8 * g:8 * g + 8], oh2[:, :, :], ohg[:, :, g:g + 1].to_broadcast([128, NT, 8]), ALU.mult),
                r=["oh2", "ohg"], w=[f"A2_{g}"])
        A1N = [f"A1_{g}" for g in range(4)]
        A2N = [f"A2_{g}" for g in range(4)]
        P.op("dve", lambda e: e.tensor_tensor(Ab[:, :, :], A1[:, :, :], A2[:, :, :], ALU.add), r=A1N + A2N, w=["Ab"])
        bin_, bi_ = bank()
        bcn_, bc_ = bank()
        Abf = Ab[:, :, :].rearrange("p a b -> p (a b)")
        P.op("pe", lambda e: e.matmul(bi_[:, :], tri[:, :], Abf, start=True, stop=True), r=["tri", "Ab"], w=[bin_])
        P.op("pe", lambda e: e.matmul(bc_[:, :], ones[:, :], Abf, start=True, stop=True), r=["ones", "Ab"], w=[bcn_])
        cnt3 = bc_[:, :].rearrange("p (a b) -> p a b", a=NT)
        P.op("dve", lambda e: e.memset(pre[:, 0, :], 0.0), w=["pre"])
        for i in range(NT):
            P.op("dve", lambda e, i=i: e.tensor_tensor(pre[:, i + 1, :], pre[:, i, :], cnt3[:, i, :], ALU.add),
                 r=["pre", bcn_], w=["pre"])
        tot = pre[:, NT, :]
        thr = cst[:, 0:16]
        sidx = cst[:, 16:80]
        P.op("dve", lambda e: e.tensor_tensor(cmpa[:, :, :], tot.unsqueeze(2).to_broadcast([128, 32, 16]),
                                              thr.unsqueeze(1).to_broadcast([128, 32, 16]), ALU.is_gt),
             r=["pre", "cst"], w=["cmpa"])
        P.op("dve", lambda e: e.reduce_sum(ns[:, :], cmpa[:, :, :], AX.X), r=["cmpa"], w=["ns"])
        cur, cur_n, oth, oth_n = ns, "ns", scA, "scA"
        for d in (1, 2, 4, 8, 16):
            P.op("dve", lambda e, d=d, cur=cur, oth=oth: e.tensor_tensor(oth[:, d:32], cur[:, d:32], cur[:, 0:32 - d],
                                                                         ALU.add), r=[cur_n], w=[oth_n + "t"])
            P.op("dve", lambda e, d=d, cur=cur, oth=oth: e.tensor_copy(oth[:, 0:d], cur[:, 0:d]),
                 r=[cur_n], w=[oth_n + "h"])
            nxt, nxt_n = (scB, "scB") if oth is scA else (scA, "scA")
            cur, cur_n, oth, oth_n = oth, oth_n, nxt, nxt_n
            cur_n_list = [cur_n + "t", cur_n + "h"]
            P.lastw[cur_n] = max((P.lastw[cur_n + "t"], P.lastw[cur_n + "h"]), key=lambda t: t[1])
            P.readers[cur_n] = []
        incl, incl_n = cur, cur_n
        P.op("dve", lambda e: e.tensor_tensor(sbase[:, :], incl[:, :], ns[:, :], ALU.subtract),
             r=[incl_n, "ns"], w=["sbase"])
        P.op("dve", lambda e: e.tensor_tensor(cmpb[:, :, :], incl[:, :].unsqueeze(1).to_broadcast([128, 64, 32]),
                                              sidx.unsqueeze(2).to_broadcast([128, 64, 32]), ALU.is_le),
             r=[incl_n, "cst"], w=["cmpb"])
        P.op("dve", lambda e: e.reduce_sum(esf[:, :], cmpb[:, :, :], AX.X), r=["cmpb"], w=["esf"])
        P.op("dve", lambda e: e.tensor_scalar(esf[:, :], esf[:, :], 128.0, cst[:, 80:81], ALU.mult, ALU.add),
             r=["esf", "cst"], w=["esf"])
        P.op("dve", lambda e: e.tensor_copy(widx[:, :], esf[:, :]), r=["esf"], w=["widx"])
        P.op("dve", lambda e: e.tensor_tensor(R[:, :, :], bi_[:, :].rearrange("p (a b) -> p a b", a=NT),
                                              pre[:, 0:NT, :], ALU.add), r=[bin_, "pre"], w=["R"])
        P.op("dve", lambda e: e.scalar_tensor_tensor(
            R[:, :, :], sbase[:, :].unsqueeze(1).to_broadcast([128, NT, 32]), float(CAP), R[:, :, :], ALU.mult, ALU.add),
            r=["sbase", "R"], w=["R"])
        for k_, (Ak, AkN) in enumerate(((A1, A1N), (A2, A2N))):
            P.op("dve", lambda e, Ak=Ak: e.tensor_tensor(prod[:, :, :], Ak[:, :, :], R[:, :, :], ALU.mult),
                 r=AkN + ["R"], w=["prod"])
            P.op("dve", lambda e, k_=k_: e.reduce_sum(posf[:, :, k_], prod[:, :, :], AX.X), r=["prod"], w=[f"posf{k_}"])
        P.op("dve", lambda e: e.tensor_copy(pos_i[:, :], posf[:, :, :].rearrange("p a b -> p (a b)")),
             r=["posf0", "posf1"], w=["pos_i"])

        xsb = [sb(f"xsb{i}", [128, D], BF16) for i in range(3)]
        xT = [sb(f"xT{i}", [128, 8, 128], BF16) for i in range(2)]
        wgu = [sb(f"wgu{i}", [128, 8, 512], BF16) for i in range(3)]
        wdn = [sb(f"wdn{i}", [128, 2, 1024], BF16) for i in range(3)]
        sg = [sb(f"sg{i}", [128, DE], F32) for i in range(2)]
        actb = [sb(f"actb{i}", [128, DE], BF16) for i in range(2)]
        actT = [sb(f"actT{i}", [128, 2, 128], BF16) for i in range(2)]
        ysb = [sb(f"ysb{i}", [128, D], BF16) for i in range(2)]
        y0 = [sb(f"y0_{i}", [128, D], BF16) for i in range(5)]
        y1 = [sb(f"y1_{i}", [128, D], BF16) for i in range(5)]
        bt = [sb(f"bt{i}", [128, D], F32) for i in range(5)]
        ot = [sb(f"ot{i}", [128, D], F32) for i in range(5)]
        vbc2 = sb("vbc2", [128, 2, D], F32)
        for j in (5, 6):
            P.dma("sp", lambda e, j=j: e.dma_start(
                out=vbc2[:, j - 5, :], in_=dram_ap(vecs_d, j * D, [[0, 128], [1, D]])), w=[f"vbc2_{j - 5}"])

        breg = {}

        def wfetch(S):
            wi = S % 3
            for nm, dst, src in ((f"wgu{wi}_0", wgu[wi][:, 0:4, :].rearrange("p k f -> p (k f)"), wgu0_d),
                                 (f"wgu{wi}_1", wgu[wi][:, 4:8, :].rearrange("p k f -> p (k f)"), wgu1_d),
                                 (f"wdn{wi}", wdn[wi][:, :, :].rearrange("p k f -> p (k f)"), wdn_d)):
                def fetch(e, dst=dst, src=src):
                    if "b" not in breg:
                        breg["b"] = e.alloc_register("wbound")
                        e.reg_mov(breg["b"], NEXP * 128 - 1)
                    return e.indirect_dma_start(
                        out=dst, out_offset=None, in_=src[:, :, :].rearrange("e p f -> (e p) f"),
                        in_offset=bass.IndirectOffsetOnAxis(ap=widx[:, S:S + 1], axis=0),
                        bounds_check=breg["b"], oob_is_err=False)
                P.dma("pool", fetch, r=["widx"], w=[nm])

        def xload(hs_):
            xi = hs_ % 3
            P.dma("sp", lambda e: e.dma_start(out=xsb[xi][:, :], in_=xs_d[hs_ * 128:(hs_ + 1) * 128, :]),
                  r=XS, w=[f"xsb{xi}"])

        GUB = [("psF0", psF[:, 0:512]), ("psF1", psF[:, 512:1024])]
        YP = [(("psF2", "psF3"), psF[:, 1024:2048]), (("psF4", "psF5"), psF[:, 2048:3072])]

        def slot_s1(hs_):
            S = hs_ // HPS
            wi = S % 3
            si = hs_ % 2
            if hs_ + 2 < NSLOT * HPS:
                xload(hs_ + 2)
            transposes(xsb[hs_ % 3], f"xsb{hs_ % 3}", si, 128, 8)
            P.op("act", lambda e: e.copy(xT[si][:, :, :], psT[si][:, :].rearrange("p (k t) -> p k t", k=8)),
                 r=[f"psT{si}"], w=[f"xT{si}"])

        def slot_s1b(hs_):
            S = hs_ // HPS
            wi = S % 3
            si = hs_ % 2
            bn, b = GUB[si]
            P.op("pe", [lambda e, k=k: e.matmul(b[:, :], xT[si][:, k, :], wgu[wi][:, k, :],
                                                start=(k == 0), stop=(k == 7)) for k in range(8)],
                 r=[f"xT{si}", f"wgu{wi}_0", f"wgu{wi}_1"], w=[bn])
            P.op("act", lambda e: e.activation(sg[si][:, :], b[:, 0:DE], AF.Silu), r=[bn], w=[f"sg{si}"])
            P.op("dve", lambda e: e.tensor_tensor(actb[si][:, :], b[:, DE:2 * DE], sg[si][:, :], ALU.mult),
                 r=[bn, f"sg{si}"], w=[f"actb{si}"])

        def slot_s2(hs_):
            S = hs_ // HPS
            wi = S % 3
            si = hs_ % 2
            ps = psT[si]
            P.op("pe", [lambda e, c=c: e.transpose(ps[:, c * 128:(c + 1) * 128], actb[si][:, c * 128:(c + 1) * 128],
                                                   ident[:, :]) for c in range(2)],
                 r=[f"actb{si}", "ident"], w=[f"psT{si}"])
            P.op("act", lambda e: e.copy(actT[si][:, :, :].rearrange("p c t -> p (c t)"), ps[:, 0:256]),
                 r=[f"psT{si}"], w=[f"actT{si}"])
            (yn0, yn1), py = YP[si]
            fns = []
            for hf in range(2):
                for c in range(2):
                    fns.append(lambda e, hf=hf, c=c: e.matmul(
                        py[:, hf * 512:(hf + 1) * 512], actT[si][:, c, :], wdn[wi][:, c, hf * 512:(hf + 1) * 512],
                        start=(c == 0), stop=(c == 1)))
            P.op("pe", fns, r=[f"actT{si}", f"wdn{wi}"], w=[yn0, yn1])
            P.op("dve", lambda e: e.tensor_copy(ysb[si][:, :], py[:, :]), r=[yn0, yn1], w=[f"ysb{si}"])
            P.dma("sp", lambda e: e.dma_start(out=ys_d[hs_ * 128:(hs_ + 1) * 128, :], in_=ysb[si][:, :]),
                  r=[f"ysb{si}"], w=[f"ys_d{hs_}"], chan=f"yst{si}")

        wfetch(0)
        wfetch(1)
        wfetch(2)
        for i in range(NT):
            for k_ in range(2):
                c = 2 * i + k_
                P.dma("pool", lambda e, i=i, c=c: e.indirect_dma_start(
                    out=xs_d[:, :], out_offset=bass.IndirectOffsetOnAxis(ap=pos_i[:, c:c + 1], axis=0),
                    in_=h1b_all[:, i, :], in_offset=None), r=[f"h1b{i}", "pos_i"] + XZ, w=[f"xs_s{c}"],
                    chan=f"sc{c % 4}")
        XS = [f"xs_s{c}" for c in range(2 * NT)]

        xload(0)
        xload(1)
        NH = NSLOT * HPS
        slot_s1(0)
        slot_s1(1)
        slot_s1b(0)
        for hs_ in range(NH):
            if hs_ + 2 < NH:
                slot_s1(hs_ + 2)
            if hs_ + 1 < NH:
                slot_s1b(hs_ + 1)
            slot_s2(hs_)
            if hs_ % HPS == HPS - 1 and hs_ // HPS + 3 < NSLOT:
                wfetch(hs_ // HPS + 3)
        YS = [f"ys_d{hs_}" for hs_ in range(NSLOT * HPS)]

        def c_prefetch(i):
            oi = i % 5
            P.dma("sp", lambda e: e.dma_start(out=bt[oi][:, :], in_=base_d[i * 128:(i + 1) * 128, :]),
                  r=[f"base_d{i}"], w=[f"bt{oi}"])
            for k_, (yy, yn) in enumerate(((y0[oi], f"y0_{oi}"), (y1[oi], f"y1_{oi}"))):
                c = 2 * i + k_
                P.dma("pool", lambda e, yy=yy, c=c: e.indirect_dma_start(
                    out=yy[:, :], out_offset=None, in_=ys_d[:, :],
                    in_offset=bass.IndirectOffsetOnAxis(ap=pos_i[:, c:c + 1], axis=0)),
                    r=YS + ["pos_i"], w=[yn])

        junk = [sb("junk0", [128, D], F32)] * 2
        st2 = [sb(f"st2_{i}", [128, 8], F32) for i in range(5)]

        def layer_norm_act(src, src_n, tmp, tmp_n, dst, dst_n, j):
            st = st2[j]
            sn = f"st2_{j}"
            P.op("act", lambda e: e.activation(junk[0][:, :], src[:, :], AF.Identity, accum_out=st[:, 0:1]),
                 r=[src_n], w=["junk0", sn + "s1"])
            P.op("act", lambda e: e.activation(junk[1][:, :], src[:, :], AF.Square, accum_out=st[:, 1:2]),
                 r=[src_n], w=["junk0", sn + "s2"])
            P.op("pool", lambda e: e.tensor_scalar(st[:, 2:3], st[:, 0:1], 1.0 / D, None, ALU.mult),
                 r=[sn + "s1"], w=[sn + "mean"])
            P.op("pool", lambda e: e.tensor_tensor(st[:, 3:4], st[:, 2:3], st[:, 2:3], ALU.mult),
                 r=[sn + "mean"], w=[sn + "msq"])
            P.op("pool", lambda e: e.tensor_scalar(st[:, 4:5], st[:, 1:2], 1.0 / D, LN_EPS, ALU.mult, ALU.add),
                 r=[sn + "s2"], w=[sn + "ve"])
            P.op("pool", lambda e: e.tensor_tensor(st[:, 4:5], st[:, 4:5], st[:, 3:4], ALU.subtract),
                 r=[sn + "ve", sn + "msq"], w=[sn + "ve"])
            P.op("pool", lambda e: e.tensor_tensor(st[:, 5:6], st[:, 4:5], neghalf[:, :], ALU.pow),
                 r=[sn + "ve", "neghalf"], w=[sn + "rs"])
            P.op("dve", lambda e: e.scalar_tensor_tensor(tmp[:, :], src[:, :], st[:, 2:3], vbc2[:, 0, :],
                                                         ALU.subtract, ALU.mult),
                 r=[src_n, sn + "mean", "vbc2_0"], w=[tmp_n])
            P.op("dve", lambda e: e.scalar_tensor_tensor(dst[:, :], tmp[:, :], st[:, 5:6], vbc2[:, 1, :],
                                                         ALU.mult, ALU.add),
                 r=[tmp_n, sn + "rs", "vbc2_1"], w=[dst_n])

        def final_tile(i):
            oi = i % 5
            P.op("dve", lambda e: e.scalar_tensor_tensor(bt[oi][:, :], y0[oi][:, :], w12[:, 0, i:i + 1], bt[oi][:, :],
                                                         ALU.mult, ALU.add), r=[f"y0_{oi}", "w1", f"bt{oi}"], w=[f"bt{oi}"])
            P.op("dve", lambda e: e.scalar_tensor_tensor(ot[oi][:, :], y1[oi][:, :], w12[:, 1, i:i + 1], bt[oi][:, :],
                                                         ALU.mult, ALU.add), r=[f"y1_{oi}", "w2", f"bt{oi}"], w=[f"ot{oi}"])
            if i + 4 < NT:
                c_prefetch(i + 4)
            layer_norm_act(ot[oi], f"ot{oi}", junk[0], "junk0", ot[oi], f"ot{oi}", oi)
            P.dma("sp", lambda e: e.dma_start(out=out_d[i * 128:(i + 1) * 128, :], in_=ot[oi][:, :]),
                  r=[f"ot{oi}"], chan=f"ost{oi}")

        c_prefetch(0)
        c_prefetch(1)
        c_prefetch(2)
        c_prefetch(3)
        for i in range(NT):
            final_tile(i)

    P.final_wait("sp")
    _DBG["P"] = P

    sems = {}
    for sk in P.count:
        sems[sk] = nc.alloc_semaphore(sk)
    with nc.Block() as block:
        @block.tensor
        def _(e):
            P.emit("pe", e, sems)

        @block.scalar
        def _(e):
            P.emit("act", e, sems)

        @block.vector
        def _(e):
            P.emit("dve", e, sems)

        @block.gpsimd
        def _(e):
            P.emit("pool", e, sems)

        @block.sync
        def _(e):
            P.emit("sp", e, sems)
    return nc


def _prep_inputs(x, p, ln_in_g, ln_in_b, w_in, conv_w, conv_b, pool_w, pool_scale, w_out, ln1_g, ln1_b,
                 w_rg, b_rg, w_re, b_re, w_gate, w_up, w_down, w_pg, b_pg, w_ple, ln2_g, ln2_b):
    f = np.float32
    x = np.asarray(x, f)
    p = np.asarray(p, f)[0]
    B, S, _ = x.shape
    vecs = np.stack([np.asarray(v, f).reshape(-1) for v in
                     (ln_in_g, ln_in_b, ln1_g, ln1_b, b_pg, ln2_g, ln2_b)], axis=0)
    wr = np.concatenate([np.asarray(w_rg, f)[0]] + [np.asarray(w_re, f)[0, g] for g in range(4)], axis=1)
    br = np.concatenate([np.asarray(b_rg, f)[0]] + [np.asarray(b_re, f)[0, g] for g in range(4)], axis=0)
    poolw = np.ascontiguousarray(np.asarray(pool_w, f)[0].transpose(1, 0, 2)).reshape(128, 512)
    cw = np.asarray(conv_w, f)[0].reshape(3, 4, 128)
    chv = np.concatenate([
        cw.transpose(2, 1, 0).reshape(128, 12),
        np.asarray(conv_b, f)[0].reshape(4, 128).T,
        np.asarray(pool_scale, f)[0].reshape(4, 128).T], axis=1)
    wg = np.asarray(w_gate, f)[0].reshape(NEXP, 8, 128, DE)
    wu = np.asarray(w_up, f)[0].reshape(NEXP, 8, 128, DE)
    wgu = np.concatenate([wg, wu], axis=3).transpose(0, 2, 1, 3).reshape(NEXP, 128, 8 * 512)
    wgu = np.ascontiguousarray(wgu)
    wdn = np.ascontiguousarray(
        np.asarray(w_down, f)[0].reshape(NEXP, 2, 128, D).transpose(0, 2, 1, 3)).reshape(NEXP, 128, 2 * D)
    shared = dict(
        ident=np.eye(128, dtype=f), vecs=np.ascontiguousarray(vecs), br=np.ascontiguousarray(br),
        w_in=np.ascontiguousarray(np.asarray(w_in, f)[0]), w_out=np.ascontiguousarray(np.asarray(w_out, f)[0]),
        w_pg=np.ascontiguousarray(np.asarray(w_pg, f)[0]), w_ple=np.ascontiguousarray(np.asarray(w_ple, f)[0]),
        wr=np.ascontiguousarray(wr), poolw=poolw, chv=np.ascontiguousarray(chv), wgu0=np.ascontiguousarray(wgu[:, :, 0:2048]),
        wgu1=np.ascontiguousarray(wgu[:, :, 2048:4096]), wdn=wdn,
        tri=np.triu(np.ones((128, 128), f), 1),
        zsrc=np.zeros((1024, D), ml_dtypes.bfloat16),
        cst=np.ascontiguousarray(np.concatenate([np.broadcast_to(np.concatenate(
            [np.arange(16, dtype=f) * float(CAP), np.arange(64, dtype=f)])[None, :], (128, 80)),
            np.arange(128, dtype=f)[:, None],
            (np.arange(NSLOT * HPS, dtype=f)[None, :] * 128.0 + np.arange(128, dtype=f)[:, None]),
            np.broadcast_to(np.array([0.0, -128.0], f)[None, :], (128, 2))], axis=1)))
    in_maps = []
    for c in range(NCORES):
        b, half = c // 2, c % 2
        s0 = half * TOK
        xl = np.zeros((TOK + HALO, D), f)
        if half == 0:
            xl[HALO:] = x[b, 0:TOK]
            hm = np.zeros((128, HALO), f)
            ic = np.stack([1.0 / np.minimum(np.arange(HALO) + 1, w) for w in POOL_W]).astype(f)
        else:
            xl[:] = x[b, s0 - HALO:s0 + TOK]
            hm = np.ones((128, HALO), f)
            ic = np.stack([np.full(HALO, 1.0 / w) for w in POOL_W]).astype(f)
        icb = np.ascontiguousarray(np.broadcast_to(ic.reshape(1, 4 * HALO), (128, 4 * HALO))).astype(f)
        m = dict(shared)
        m.update(x=xl, p=np.ascontiguousarray(p[b, s0:s0 + TOK]), hmask=hm, invcnt=icb)
        in_maps.append(m)
    return in_maps, (B, S)


_NC_CACHE = {}


def kernel(**inputs):
    in_maps, (B, S) = _prep_inputs(**inputs)
    if "nc" not in _NC_CACHE:
        _NC_CACHE["nc"] = build_nc()
    nc = _NC_CACHE["nc"]
    res = run_bass_kernel_spmd(nc, in_maps, core_ids=list(range(NCORES)))
    out = np.empty((B, S, D), np.float32)
    for c in range(NCORES):
        b, half = c // 2, c % 2
        out[b, half * TOK:(half + 1) * TOK] = res.results[c]["out"]
    return out
```

```python
import numpy as np
import ml_dtypes
import concourse.bass as bass
import concourse.mybir as mybir
from concourse.bass_utils import run_bass_kernel_spmd

F32 = mybir.dt.float32
BF16 = mybir.dt.bfloat16
I32 = mybir.dt.int32
ALU = mybir.AluOpType
AF = mybir.ActivationFunctionType
AX = mybir.AxisListType

NCORES = 8
SCHED_A = ['A', 'A', 'B0', 'A', 'B0', 'B1', 'A', 'A', 'A', 'A', 'B1', 'A', 'B0', 'B1', 'A', 'B0', 'A', 'B0', 'B1', 'B1',
           'A']
_DBG = {}
CAP = 256
HPS = CAP // 128
NSLOT = 48
ZERO_INIT = False
D = 1024
TOK = 2048
HALO = 16
NT = TOK // 128
ST = 256
NST = TOK // ST
TPS = ST // 128
INP = 2048
NEXP = 32
DE = 256
PLE = 256
LN_EPS = 1e-5
ALPHA = 2.0 ** 0.25
POOL_W = (2, 4, 8, 16)
SB_BASE = 16512
SB_END = 229376


class Prog:
    ENG = ("pe", "act", "dve", "pool", "sp")

    def __init__(self):
        self.lists = {e: [] for e in self.ENG}
        self.count = {}
        self.waited = {e: {} for e in self.ENG}
        self.lastw = {}
        self.readers = {}

    def _waits(self, eng, r, w, is_dma):
        deps = []
        for n in list(r) + list(w):
            t = self.lastw.get(n)
            if t is not None:
                deps.append((t, True))
        for n in w:
            for t in self.readers.get(n, ()):
                deps.append((t, False))
        need = {}
        for (sk, v, src), is_w in deps:
            if src == eng and not is_dma:
                if eng == "pe":
                    continue
            if need.get(sk, 0) < v:
                need[sk] = v
        waits = []
        for sk, v in need.items():
            if self.waited[eng].get(sk, 0) < v:
                self.waited[eng][sk] = v
                waits.append((sk, v))
        return waits

    def _commit(self, tok, r, w):
        for n in w:
            self.lastw[n] = tok
            self.readers[n] = []
        for n in r:
            if n not in w:
                self.readers.setdefault(n, []).append(tok)

    def op(self, eng, fns, r=(), w=()):
        if not isinstance(fns, (list, tuple)):
            fns = [fns]
        waits = self._waits(eng, r, w, False)
        sk = "s_" + eng
        self.count[sk] = self.count.get(sk, 0) + 1
        tok = (sk, self.count[sk], eng)
        self.lists[eng].append((list(fns), waits, (sk, 1)))
        self._commit(tok, r, w)
        return tok

    def dma(self, q, fn, r=(), w=(), chan=None):
        waits = self._waits(q, r, w, True)
        if chan is None:
            chan = (list(w) + list(r))[0]
        sk = "d_" + chan
        self.count[sk] = self.count.get(sk, 0) + 16
        tok = (sk, self.count[sk], "dma")
        self.lists[q].append(([fn], waits, (sk, 16)))
        self._commit(tok, r, w)
        return tok

    def barrier(self):
        for e in self.ENG:
            waits = []
            for sk, v in self.count.items():
                if sk == "s_" + e and e == "pe":
                    continue
                if self.waited[e].get(sk, 0) < v:
                    self.waited[e][sk] = v
                    waits.append((sk, v))
            if waits:
                self.lists[e].append(([], waits, None))

    def final_wait(self, eng="sp"):
        waits = []
        for sk, v in self.count.items():
            if self.waited[eng].get(sk, 0) < v:
                self.waited[eng][sk] = v
                waits.append((sk, v))
        self.lists[eng].append(([], waits, None))

    def emit(self, eng, handle, sems):
        for fns, waits, inc in self.lists[eng]:
            for sk, v in waits:
                handle.wait_ge(sems[sk], v)
            ins = None
            for fn in fns:
                ins = fn(handle)
            if inc is not None:
                ins.then_inc(sems[inc[0]], inc[1])


class SbAlloc:
    def __init__(self, nc):
        self.nc = nc
        self.top = SB_BASE
        self.n = 0

    def mark(self):
        return self.top

    def reset(self, m):
        self.top = m

    def __call__(self, name, shape, dtype):
        esz = 4 if dtype in (F32, I32) else 2
        nbytes = esz
        for s in shape[1:]:
            nbytes *= s
        off = (self.top + 31) // 32 * 32
        assert off + nbytes <= SB_END, f"SBUF overflow at {name}: {off + nbytes}"
        self.top = off + nbytes
        self.n += 1
        return self.nc.alloc_sbuf_tensor_at(f"{name}_{self.n}", list(shape), dtype, offset=off)


def dram_ap(t, offset, pattern):
    return bass.AP(t, offset, [list(p) for p in pattern])


def build_nc(debug_out=None):
    nc = bass.Bass("TRN2", target_bir_lowering=False)
    P = Prog()
    sb = SbAlloc(nc)

    def din(name, shape, dt=F32):
        return nc.dram_tensor(name, list(shape), dt, kind="ExternalInput")

    x_d = din("x", [TOK + HALO, D])
    p_d = din("p", [TOK, PLE])
    hmask_d = din("hmask", [128, HALO])
    invcnt_d = din("invcnt", [128, 4 * HALO])
    ident_d = din("ident", [128, 128])
    vecs_d = din("vecs", [7, D])
    br_d = din("br", [36])
    w_in_d = din("w_in", [D, INP])
    w_out_d = din("w_out", [D, D])
    w_pg_d = din("w_pg", [D, D])
    w_ple_d = din("w_ple", [PLE, D])
    wr_d = din("wr", [D, 36])
    poolw_d = din("poolw", [128, 4 * 128])
    chv_d = din("chv", [128, 20])
    wgu0_d = din("wgu0", [NEXP, 128, 4 * 512])
    wgu1_d = din("wgu1", [NEXP, 128, 4 * 512])
    wdn_d = din("wdn", [NEXP, 128, 2 * 1024])
    tri_d = din("tri", [128, 128])
    cst_d = din("cst", [128, 179])
    zsrc_d = din("zsrc", [1024, D], BF16)
    out_d = nc.dram_tensor("out", [TOK, D], F32, kind="ExternalOutput")
    base_d = nc.dram_tensor("base_scr", [TOK, D], F32)
    xs_d = nc.dram_tensor("xs_scr", [NSLOT * CAP, D], BF16)
    ys_d = nc.dram_tensor("ys_scr", [NSLOT * CAP, D], BF16)

    psT = [nc.alloc_psum_tensor(f"psT{i}", [128, 1024], BF16) for i in range(2)]
    psF = nc.alloc_psum_tensor("psF", [128, 6 * 512], F32)
    bank_rr = [0]
    pair_rr = [0]

    pool_lo = [0]

    def bank():
        b = bank_rr[0] % 6
        bank_rr[0] += 1
        return f"psF{b}", psF[:, b * 512:(b + 1) * 512]

    def bank_a1():
        b = pool_lo[0] % 3
        pool_lo[0] += 1
        return f"psF{b}", psF[:, b * 512:(b + 1) * 512]

    def pair():
        b = pair_rr[0] % 3
        pair_rr[0] += 1
        return (f"psF{2 * b}", f"psF{2 * b + 1}"), psF[:, b * 1024:(b + 1) * 1024]

    ident = sb("ident", [128, 128], BF16)
    vbc = sb("vbc", [128, 4, D], F32)
    bpg_bf = sb("bpg_bf", [1, D], BF16)
    ones_row = sb("ones_row", [1, 128], BF16)
    br_bc = sb("br_bc", [128, 36], F32)
    chv = sb("chv", [128, 20], F32)
    hmask = sb("hmask", [128, HALO], F32)
    invcnt = sb("invcnt", [128, 4, HALO], F32)
    neghalf = sb("neghalf", [128, 1], F32)
    L_all = sb("L_all", [128, NT, 36], F32)
    h1b_all = sb("h1b_all", [128, NT, D], BF16)
    w12 = sb("w12", [128, 2, NT], F32)
    pos_i = sb("pos_i", [128, 2 * NT], I32)
    widx = sb("widx", [128, 64], I32)
    zt = sb("zt", [128, D], BF16) if ZERO_INIT else None
    NLN = 8
    lnst = [sb(f"lnst{j}", [128, 16], F32) for j in range(NLN)]
    ln_rr = [0]
    mark_persist = sb.mark()

    P.dma("pool", lambda e: e.dma_start(out=ident[:, :], in_=ident_d[:, :]), w=["ident"])
    for j in range(4):
        P.dma("sp", lambda e, j=j: e.dma_start(
            out=vbc[:, j, :], in_=dram_ap(vecs_d, j * D, [[0, 128], [1, D]])), w=[f"vbc{j}"])
    P.dma("pool", lambda e: e.dma_start(out=bpg_bf[0:1, :], in_=vecs_d[4:5, :]), w=["bpg_bf"])
    P.op("pool", lambda e: e.memset(ones_row[0:1, :], 1.0), w=["ones_row"])
    P.dma("sp", lambda e: e.dma_start(out=br_bc[:, :], in_=dram_ap(br_d, 0, [[0, 128], [1, 36]])), w=["br_bc"])
    P.dma("sp", lambda e: e.dma_start(out=chv[:, :], in_=chv_d[:, :]), w=["chv"])
    P.dma("sp", lambda e: e.dma_start(out=hmask[:, :], in_=hmask_d[:, :]), w=["hmask"])
    P.dma("sp", lambda e: e.dma_start(out=invcnt[:, :, :].rearrange("p g t -> p (g t)"), in_=invcnt_d[:, :]),
          w=["invcnt"])
    P.op("pool", lambda e: e.memset(neghalf[:, :], -0.5), w=["neghalf"])
    XZ = []
    if False:
        P.op("pool", lambda e: e.memset(zt[:, :], 0.0), w=["zt"])
        for j in range(NSLOT * HPS):
            P.dma("sp", lambda e, j=j: e.dma_start(out=xs_d[j * 128:(j + 1) * 128, :], in_=zt[:, :]),
                  r=["zt"], w=[f"xs_z{j}"], chan=f"xz{j % 4}")
            XZ.append(f"xs_z{j}")

    def layer_norm(src, src_n, tmp, tmp_n, dst, dst_n, gi, bi, np_=128, dstb=None, dstb_n=None, vb=None):
        vb = vbc if vb is None else vb
        j = ln_rr[0] % NLN
        ln_rr[0] += 1
        st = lnst[j]
        sn = f"lnst{j}"
        P.op("dve", lambda e: e.bn_stats(st[:np_, 0:6], src[:np_, 0:512]), r=[src_n], w=[sn + "a"])
        P.op("dve", lambda e: e.bn_stats(st[:np_, 6:12], src[:np_, 512:1024]), r=[src_n], w=[sn + "b"])
        P.op("dve", lambda e: e.bn_aggr(st[:np_, 12:14], st[:np_, 0:12]), r=[sn + "a", sn + "b"], w=[sn + "mv"])
        P.op("pool", lambda e: e.tensor_scalar(st[:np_, 14:15], st[:np_, 13:14], LN_EPS, None, ALU.add),
             r=[sn + "mv"], w=[sn + "ve"])
        P.op("pool", lambda e: e.tensor_tensor(st[:np_, 15:16], st[:np_, 14:15], neghalf[:np_, :], ALU.pow),
             r=[sn + "ve", "neghalf"], w=[sn + "rs"])
        P.op("dve", lambda e: e.scalar_tensor_tensor(tmp[:np_, :], src[:np_, :], st[:np_, 12:13],
                                                     vb[:np_, gi, :], ALU.subtract, ALU.mult),
             r=[src_n, sn + "mv", f"vbc{gi}"], w=[tmp_n])
        P.op("dve", lambda e: e.scalar_tensor_tensor(dst[:np_, :], tmp[:np_, :], st[:np_, 15:16],
                                                     vb[:np_, bi, :], ALU.mult, ALU.add),
             r=[tmp_n, sn + "rs", f"vbc{bi}"], w=[dst_n])
        if dstb is not None:
            P.op("act", lambda e: e.copy(dstb[:np_, :], dst[:np_, :]), r=[dst_n], w=[dstb_n])

    w_in_sb = sb("w_in_sb", [128, 8, INP], BF16)
    w_out_sb = sb("w_out_sb", [128, 8, D], BF16)
    w_pg_sb = sb("w_pg_sb", [128, 8, D], BF16)
    w_ple_sb = sb("w_ple_sb", [128, 2, D], BF16)
    wr_sb = sb("wr_sb", [128, 8, 36], BF16)
    poolw_sb = sb("poolw_sb", [128, 4, 128], BF16)
    zc = sb("zc", [128, 4, HALO], F32)
    vc = sb("vc", [128, 4, HALO], F32)
    xt = [sb(f"xt{i}", [128, D], F32) for i in range(2)]
    tmp1 = [sb(f"tmp1_{i}", [128, D], F32) for i in range(2)]
    tmp2 = sb("tmp2", [128, D], F32)
    tmp2a = sb("tmp2a", [128, D], F32)
    hbuf = [sb(f"h{i}", [128, D], F32) for i in range(4)]
    hb = [sb(f"hb{i}", [128, D], BF16) for i in range(2)]
    hT = [sb(f"hT{i}", [128, 8, ST], BF16) for i in range(2)]
    hTh = sb("hTh", [128, 8, HALO], BF16)
    cg = [sb(f"cg{i}", [128, ST], F32) for i in range(2)]
    z_ext = [sb(f"z_ext{i}", [128, HALO + ST], F32) for i in range(2)]
    acc = [sb(f"acc{i}", [128, ST], F32) for i in range(2)]
    vp_ext = [sb(f"vp_ext{i}", [128, HALO + ST], F32) for i in range(2)]
    tA = sb("tA", [128, HALO + ST], F32)
    tB = sb("tB", [128, HALO + ST], F32)
    tmpc = sb("tmpc", [128, HALO], F32)
    pooledT = [sb(f"pooledT{i}", [128, ST], BF16) for i in range(2)]
    ycatT = [sb(f"ycatT{i}", [128, 8, ST], BF16) for i in range(2)]
    h1T = [sb(f"h1T{i}", [128, 8, 128], BF16) for i in range(2)]
    pb = [sb(f"pb{i}", [128, PLE], BF16) for i in range(2)]
    pT = [sb(f"pT{i}", [128, 2, 128], BF16) for i in range(2)]

    for k in range(8):
        for hf in range(2):
            P.dma("pool", lambda e, k=k, hf=hf: e.dma_start(
                out=w_in_sb[:, k, hf * 1024:(hf + 1) * 1024],
                in_=w_in_d[k * 128:(k + 1) * 128, hf * 1024:(hf + 1) * 1024]), w=[f"w_in{k}_{hf}"], chan="w_in")
    P.dma("pool", lambda e: e.dma_start(out=poolw_sb[:, :, :].rearrange("p g d -> p (g d)"), in_=poolw_d[:, :]),
          w=["poolw"])
    def late_weights():
        for k in range(8):
            P.dma("pool", lambda e, k=k: e.dma_start(out=w_out_sb[:, k, :], in_=w_out_d[k * 128:(k + 1) * 128, :]),
                  w=[f"w_out{k}"], chan="w_out")
        for k in range(8):
            P.dma("pool", lambda e, k=k: e.dma_start(out=wr_sb[:, k, :], in_=wr_d[k * 128:(k + 1) * 128, :]),
                  w=[f"wr{k}"], chan="wr")
        for k in range(8):
            P.dma("pool", lambda e, k=k: e.dma_start(out=w_pg_sb[:, k, :], in_=w_pg_d[k * 128:(k + 1) * 128, :]),
                  w=[f"w_pg{k}"], chan="w_pg")
        for k in range(2):
            P.dma("pool", lambda e, k=k: e.dma_start(out=w_ple_sb[:, k, :], in_=w_ple_d[k * 128:(k + 1) * 128, :]),
                  w=[f"w_ple{k}"], chan="w_ple")

    W_IN = [f"w_in{k}_{hf}" for k in range(8) for hf in range(2)]
    W_OUT = [f"w_out{k}" for k in range(8)]
    W_R = [f"wr{k}" for k in range(8)]
    W_PG = [f"w_pg{k}" for k in range(8)]
    W_PLE = [f"w_ple{k}" for k in range(2)]

    def transposes(src, src_n, ps_i, np_, nchunk, csz=128):
        ps = psT[ps_i]
        fns = []
        for c in range(nchunk):
            fns.append(lambda e, c=c: e.transpose(ps[:csz, c * np_:(c + 1) * np_],
                                                  src[:np_, c * csz:(c + 1) * csz], ident[:np_, :np_]))
        P.op("pe", fns, r=[src_n, "ident"], w=[f"psT{ps_i}"])

    def halo_stage():
        P.dma("sp", lambda e: e.dma_start(out=xt[1][:HALO, :], in_=x_d[0:HALO, :]), w=["xt1"])
        layer_norm(xt[1], "xt1", tmp2, "tmp2", hbuf[3], "h3", 0, 1, np_=HALO, dstb=hb[1], dstb_n="hb1")
        transposes(hb[1], "hb1", 1, HALO, 8)
        P.op("act", lambda e: e.copy(hTh[:, :, :].rearrange("p k t -> p (k t)"), psT[1][:, 0:8 * HALO]),
             r=["psT1"], w=["hTh"])
        for q in range(4):
            bc_n, bc = bank()
            bv_n, bv = bank()
            for f, (bn, b) in ((4 + q, (bc_n, bc)), (8 + q, (bv_n, bv))):
                P.op("pe", [lambda e, k=k, f=f, b=b: e.matmul(b[:, 0:HALO], w_in_sb[:, k, f * 128:(f + 1) * 128],
                                                              hTh[:, k, :], start=(k == 0), stop=(k == 7))
                            for k in range(8)], r=["hTh"] + W_IN, w=[bn])
            P.op("act", lambda e, bc=bc: e.copy(cg[0][:, 0:HALO], bc[:, 0:HALO]), r=[bc_n], w=["cg0"])
            P.op("dve", lambda e, bv=bv: e.tensor_tensor(tmpc[:, :], bv[:, 0:HALO], cg[0][:, 0:HALO], ALU.mult),
                 r=[bv_n, "cg0"], w=["tmpc"])
            P.op("dve", lambda e, q=q: e.tensor_tensor(zc[:, q, :], tmpc[:, :], hmask[:, :], ALU.mult),
                 r=["tmpc", "hmask"], w=[f"zc{q}"])
        for g in range(4):
            bp_n, bp = bank()
            f = 12 + g
            P.op("pe", [lambda e, k=k, f=f, b=bp: e.matmul(b[:, 0:HALO], w_in_sb[:, k, f * 128:(f + 1) * 128],
                                                           hTh[:, k, :], start=(k == 0), stop=(k == 7))
                        for k in range(8)], r=["hTh"] + W_IN, w=[bp_n])
            P.op("dve", lambda e, g=g, bp=bp: e.tensor_tensor(vc[:, g, :], bp[:, 0:HALO], hmask[:, :], ALU.mult),
                 r=[bp_n, "hmask"], w=[f"vc{g}"])

    def x_prefetch(S):
        for m in range(TPS):
            i = S * TPS + m
            P.dma("sp", lambda e, i=i: e.dma_start(out=xt[i % 2][:, :],
                                                   in_=x_d[HALO + i * 128:HALO + (i + 1) * 128, :]), w=[f"xt{i % 2}"])

    def stage_a1(S):
        hs = S % 2
        for m in range(TPS):
            i = S * TPS + m
            xn = f"xt{i % 2}"
            layer_norm(xt[i % 2], xn, tmp2a, "tmp2a", hbuf[i % 4], f"h{i % 4}", 0, 1,
                       dstb=hb[i % 2], dstb_n=f"hb{i % 2}")
            if debug_out == "h":
                P.dma("sp", lambda e, i=i: e.dma_start(out=out_d[i * 128:(i + 1) * 128, :], in_=hbuf[i % 4][:, :]),
                      r=[f"h{i % 4}"], chan=f"st{i % 2}")
            yield
        if S + 1 < NST:
            x_prefetch(S + 1)
        if debug_out is None:
            for z in range(2 * S, min(2 * S + 2, (NSLOT * CAP) // 1024)):
                P.dma("sp", lambda e, z=z: e.dma_start(out=xs_d[z * 1024:(z + 1) * 1024, :], in_=zsrc_d[:, :]),
                      w=[f"xs_z{z}"], chan=f"xz{z % 2}")
                XZ.append(f"xs_z{z}")
        for m in range(TPS):
            i = S * TPS + m
            transposes(hb[i % 2], f"hb{i % 2}", i % 2, 128, 8)
            P.op("act", lambda e, i=i, m=m: e.copy(
                hT[hs][:, :, m * 128:(m + 1) * 128],
                psT[i % 2][:, :].rearrange("p (k t) -> p k t", k=8)), r=[f"psT{i % 2}"], w=[f"hT{hs}_{m}"])
        yield
        HTN = [f"hT{hs}_{m}" for m in range(TPS)]

        def inproj(f):
            bn, b = bank_a1()
            P.op("pe", [lambda e, k=k: e.matmul(b[:, 0:ST], w_in_sb[:, k, f * 128:(f + 1) * 128],
                                                hT[hs][:, k, :], start=(k == 0), stop=(k == 7))
                        for k in range(8)], r=HTN + W_IN, w=[bn])
            return bn, b

        def conv_chunk(q):
            zi = q % 2
            ze, zn, ac, an = z_ext[zi], f"z_ext{zi}", acc[zi], f"acc{zi}"
            bcn, bc_ = inproj(4 + q)
            bvn, bv_ = inproj(8 + q)
            P.op("act", lambda e, bc_=bc_: e.copy(cg[zi][:, :], bc_[:, 0:ST]), r=[bcn], w=[f"cg{zi}"])
            P.op("pool", lambda e, q=q, ze=ze: e.tensor_copy(ze[:, 0:HALO], zc[:, q, :]), r=[f"zc{q}"], w=[zn + "h"])
            P.op("dve", lambda e, bv_=bv_, ze=ze, zi=zi: e.tensor_tensor(ze[:, HALO:HALO + ST], bv_[:, 0:ST],
                                                                        cg[zi][:, :], ALU.mult),
                 r=[bvn, f"cg{zi}"], w=[zn + "b"])
            P.op("pool", lambda e, q=q, ze=ze: e.tensor_copy(zc[:, q, :], ze[:, ST:ST + HALO]),
                 r=[zn + "b"], w=[f"zc{q}"])
            P.op("pool", lambda e, q=q, ze=ze, ac=ac: e.tensor_scalar(
                ac[:, :], ze[:, HALO:HALO + ST], chv[:, 3 * q + 2:3 * q + 3], chv[:, 12 + q:13 + q],
                ALU.mult, ALU.add), r=[zn + "b", "chv"], w=[an])
            P.op("dve", lambda e, q=q, ze=ze, ac=ac: e.scalar_tensor_tensor(
                ac[:, :], ze[:, HALO - 1:HALO - 1 + ST], chv[:, 3 * q + 1:3 * q + 2], ac[:, :],
                ALU.mult, ALU.add), r=[zn + "b", zn + "h", "chv", an], w=[an])
            P.op("dve", lambda e, q=q, ze=ze, ac=ac: e.scalar_tensor_tensor(
                ac[:, :], ze[:, HALO - 2:HALO - 2 + ST], chv[:, 3 * q:3 * q + 1], ac[:, :],
                ALU.mult, ALU.add), r=[zn + "b", zn + "h", "chv", an], w=[an])
            bbn, bb_ = inproj(q)
            P.op("dve", lambda e, q=q, bb_=bb_, ac=ac: e.tensor_tensor(ycatT[hs][:, q, :], bb_[:, 0:ST], ac[:, :],
                                                                      ALU.mult),
                 r=[bbn, an], w=[f"ycatT{hs}_{q}"])

        def pool_chunk(g):
            vi = g % 2
            ve, vn = vp_ext[vi], f"vp_ext{vi}"
            bpn, bp_ = inproj(12 + g)
            P.op("pool", lambda e, g=g, ve=ve: e.tensor_copy(ve[:, 0:HALO], vc[:, g, :]), r=[f"vc{g}"], w=[vn + "h"])
            P.op("act", lambda e, bp_=bp_, ve=ve: e.copy(ve[:, HALO:HALO + ST], bp_[:, 0:ST]), r=[bpn], w=[vn + "b"])
            P.op("pool", lambda e, g=g, ve=ve: e.tensor_copy(vc[:, g, :], ve[:, ST:ST + HALO]),
                 r=[vn + "b"], w=[f"vc{g}"])
            E = HALO + ST
            P.op("pool", lambda e, ve=ve: e.tensor_tensor(tA[:, 1:E], ve[:, 1:E], ve[:, 0:E - 1], ALU.add),
                 r=[vn + "b", vn + "h"], w=["tA"])
            s_t, s_n = tA, "tA"
            if g >= 1:
                P.op("pool", lambda e: e.tensor_tensor(tB[:, 3:E], tA[:, 3:E], tA[:, 1:E - 2], ALU.add),
                     r=["tA"], w=["tB"])
                s_t, s_n = tB, "tB"
            if g >= 2:
                P.op("pool", lambda e: e.tensor_tensor(tA[:, 7:E], tB[:, 7:E], tB[:, 3:E - 4], ALU.add),
                     r=["tB"], w=["tA"])
                s_t, s_n = tA, "tA"
            if g >= 3:
                P.op("pool", lambda e: e.tensor_tensor(tB[:, 15:E], tA[:, 15:E], tA[:, 7:E - 8], ALU.add),
                     r=["tA"], w=["tB"])
                s_t, s_n = tB, "tB"
            pn = f"pooledT{vi}"
            P.op("dve", lambda e, g=g, ve=ve, s_t=s_t, vi=vi: e.scalar_tensor_tensor(
                pooledT[vi][:, :], s_t[:, HALO:E], 1.0 / POOL_W[g], ve[:, HALO:E], ALU.mult, ALU.subtract),
                r=[s_n, vn + "b"], w=[pn])
            if S == 0:
                P.op("dve", lambda e, g=g, s_t=s_t: e.tensor_tensor(tmpc[:, :], s_t[:, HALO:2 * HALO],
                                                                    invcnt[:, g, :], ALU.mult),
                     r=[s_n, "invcnt"], w=["tmpc"])
                P.op("dve", lambda e, ve=ve, vi=vi: e.tensor_tensor(pooledT[vi][:, 0:HALO], tmpc[:, :],
                                                                    ve[:, HALO:2 * HALO], ALU.subtract),
                     r=["tmpc", vn + "b", pn], w=[pn])
            bln, bl_ = bank_a1()
            P.op("pe", lambda e, g=g, vi=vi, bl_=bl_: e.matmul(bl_[:, 0:ST], poolw_sb[:, g, :], pooledT[vi][:, :],
                                                               start=True, stop=True),
                 r=[pn, "poolw"], w=[bln])
            P.op("act", lambda e, g=g, bl_=bl_: e.mul(ycatT[hs][:, 4 + g, :], bl_[:, 0:ST], chv[:, 16 + g:17 + g]),
                 r=[bln, "chv"], w=[f"ycatT{hs}_{4 + g}"])

        for q in range(4):
            conv_chunk(q)
            yield
            pool_chunk(q)
            yield

    def stage_a2(S, m):
        hs = S % 2
        YN = [f"ycatT{hs}_{c}" for c in range(8)]
        if True:
            i = S * TPS + m
            (pn0, pn1), pr = ("psF4", "psF5"), psF[:, 2048:3072]
            fns = []
            for hf in range(2):
                for k in range(8):
                    fns.append(lambda e, hf=hf, k=k: e.matmul(
                        pr[:, hf * 512:(hf + 1) * 512], ycatT[hs][:, k, m * 128:(m + 1) * 128],
                        w_out_sb[:, k, hf * 512:(hf + 1) * 512], start=(k == 0), stop=(k == 7)))
            P.op("pe", fns, r=YN + W_OUT, w=[pn0, pn1])
            t1, t1n = tmp1[i % 2], f"tmp1_{i % 2}"
            P.op("dve", lambda e, i=i, pr=pr, t1=t1: e.scalar_tensor_tensor(
                t1[:, :], hbuf[i % 4][:, :], ALPHA, pr[:, :], ALU.mult, ALU.add),
                r=[f"h{i % 4}", pn0, pn1], w=[t1n])
            yield
            layer_norm(t1, t1n, tmp2, "tmp2", t1, t1n, 2, 3, dstb=h1b_all[:, i, :], dstb_n=f"h1b{i}")
            yield
            transposes(h1b_all[:, i, :], f"h1b{i}", i % 2, 128, 8)
            P.op("act", lambda e, i=i: e.copy(
                h1T[i % 2][:, :, :],
                psT[i % 2][:, :].rearrange("p (k t) -> p k t", k=8)), r=[f"psT{i % 2}"], w=[f"h1T{i % 2}"])
            yield
            yield from stage_a3(i)

    def stage_a3(i):
        t1, t1n = tmp1[i % 2], f"tmp1_{i % 2}"
        if debug_out == "h1":
            P.dma("sp", lambda e: e.dma_start(out=out_d[i * 128:(i + 1) * 128, :], in_=t1[:, :]), r=[t1n],
                  chan=f"st{i % 2}")
        bn, b = "psF3", psF[:, 3 * 512:4 * 512]
        P.op("pe", [lambda e, k=k: e.matmul(b[:, 0:36], h1T[i % 2][:, k, :], wr_sb[:, k, :],
                                            start=(k == 0), stop=(k == 7)) for k in range(8)],
             r=[f"h1T{i % 2}"] + W_R, w=[bn])
        P.op("dve", lambda e: e.tensor_tensor(L_all[:, i, :], b[:, 0:36], br_bc[:, :], ALU.add),
             r=[bn, "br_bc"], w=[f"L{i}"])
        pi = i % 2
        P.dma("pool", lambda e: e.dma_start(out=pb[pi][:, :], in_=p_d[i * 128:(i + 1) * 128, :]), w=[f"pb{pi}"])
        transposes(pb[pi], f"pb{pi}", pi, 128, 2)
        P.op("act", lambda e: e.copy(pT[pi][:, :, :].rearrange("p c t -> p (c t)"), psT[pi][:, 0:256]),
             r=[f"psT{pi}"], w=[f"pT{pi}"])
        for hf in range(2):
            cs = slice(hf * 512, (hf + 1) * 512)
            gn, pg_ = "psF4", psF[:, 4 * 512:5 * 512]
            an, pa = "psF5", psF[:, 5 * 512:6 * 512]
            P.op("pe", [lambda e, k=k, cs=cs, pg_=pg_: e.matmul(pg_[:, :], h1T[i % 2][:, k, :], w_pg_sb[:, k, cs],
                                                               start=(k == 0), stop=False) for k in range(8)]
                 + [lambda e, cs=cs, pg_=pg_: e.matmul(pg_[:, :], ones_row[0:1, :], bpg_bf[0:1, cs],
                                                      start=False, stop=True)],
                 r=[f"h1T{i % 2}", "ones_row", "bpg_bf"] + W_PG, w=[gn])
            P.op("pe", [lambda e, c=c, cs=cs, pa=pa: e.matmul(pa[:, :], pT[pi][:, c, :], w_ple_sb[:, c, cs],
                                                             start=(c == 0), stop=(c == 1)) for c in range(2)],
                 r=[f"pT{pi}"] + W_PLE, w=[an])
            tn = f"tmp2_{hf}"
            P.op("act", lambda e, cs=cs, pg_=pg_: e.activation(tmp2[:, cs], pg_[:, :], AF.Sigmoid),
                 r=[gn], w=[tn, "tmp2"])
            P.op("dve", lambda e, cs=cs, pa=pa: e.tensor_tensor(tmp2[:, cs], pa[:, :], tmp2[:, cs], ALU.mult),
                 r=[an, tn], w=[tn])
            if hf == 0:
                yield
        P.op("dve", lambda e: e.scalar_tensor_tensor(t1[:, :], t1[:, :], ALPHA, tmp2[:, :], ALU.mult, ALU.add),
             r=[t1n, "tmp2_0", "tmp2_1"], w=[t1n, "tmp2"])
        if debug_out is None:
            P.dma("sp", lambda e: e.dma_start(out=base_d[i * 128:(i + 1) * 128, :], in_=t1[:, :]), r=[t1n],
                  w=[f"base_d{i}"], chan=f"st{i % 2}")
        yield

    def run_sched(gens, sched):
        alive = dict(gens)
        for key in sched:
            g = alive.get(key)
            if g is None:
                continue
            try:
                next(g)
            except StopIteration:
                del alive[key]
        run_rr(list(alive.values()))

    def run_rr(gens):
        gens = list(gens)
        while gens:
            for g in list(gens):
                try:
                    next(g)
                except StopIteration:
                    gens.remove(g)

    halo_stage()
    x_prefetch(0)
    run_rr([stage_a1(0)])
    late_weights()
    SCHED = list(SCHED_A)
    for S in range(NST):
        gens = {"B0": stage_a2(S, 0), "B1": stage_a2(S, 1)}
        if S + 1 < NST:
            gens["A"] = stage_a1(S + 1)
        run_sched(gens, SCHED)

    if debug_out is None:
        P.barrier()
        sb.reset(mark_persist)
        rt = {}

        def T(name, shape, dt=F32):
            rt[name] = sb("rt_" + name, shape, dt)
            return rt[name]

        LN_ = [f"L{i}" for i in range(NT)]
        tri = T("tri", [128, 128], BF16)
        ones = T("ones", [128, 128], BF16)
        cst = T("cst", [128, 179])
        P.dma("pool", lambda e: e.dma_start(out=tri[:, :], in_=tri_d[:, :]), w=["tri"])
        P.dma("sp", lambda e: e.dma_start(out=cst[:, :], in_=cst_d[:, :]), w=["cst"])
        P.op("pool", lambda e: e.memset(ones[:, :], 1.0), w=["ones"])
        gmax = T("gmax", [128, NT])
        ohg = T("ohg", [128, NT, 4])
        ex = T("ex", [128, NT, 4])
        se = T("se", [128, NT])
        gw = T("gw", [128, NT])
        el = T("el", [128, NT, 8])
        elt = T("elt", [128, NT, 8])
        m1 = T("m1", [128, NT])
        oh1 = T("oh1", [128, NT, 8])
        el2 = T("el2", [128, NT, 8])
        m2 = T("m2", [128, NT])
        oh2 = T("oh2", [128, NT, 8])
        dd = T("dd", [128, NT])
        A1 = T("A1", [128, NT, 32])
        A2 = T("A2", [128, NT, 32])
        Ab = T("Ab", [128, NT, 32], BF16)
        pre = T("pre", [128, NT + 1, 32])
        cmpa = T("cmpa", [128, 32, 16])
        ns = T("ns", [128, 32])
        scA = T("scA", [128, 32])
        scB = T("scB", [128, 32])
        sbase = T("sbase", [128, 32])
        R = T("R", [128, NT, 32])
        prod = T("prod", [128, NT, 32])
        posf = T("posf", [128, NT, 2])
        cmpb = T("cmpb", [128, 64, 32])
        esf = T("esf", [128, 64])

        def bc(t, n):
            return t[:, :].unsqueeze(2).to_broadcast([128, NT, n])

        gl = L_all[:, :, 0:4]
        P.op("dve", lambda e: e.reduce_max(gmax[:, :], gl, AX.X), r=LN_, w=["gmax"])
        P.op("dve", lambda e: e.tensor_tensor(ohg[:, :, :], gl, bc(gmax, 4), ALU.is_equal), r=LN_ + ["gmax"], w=["ohg"])
        P.op("dve", lambda e: e.tensor_tensor(ex[:, :, :], gl, bc(gmax, 4), ALU.subtract), r=LN_ + ["gmax"], w=["ex"])
        P.op("act", lambda e: e.activation(ex[:, :, :], ex[:, :, :], AF.Exp), r=["ex"], w=["ex"])
        P.op("dve", lambda e: e.reduce_sum(se[:, :], ex[:, :, :], AX.X), r=["ex"], w=["se"])
        P.op("dve", lambda e: e.reciprocal(gw[:, :], se[:, :]), r=["se"], w=["gw"])
        for g in range(4):
            dst = el if g == 0 else elt
            dn = "el" if g == 0 else "elt"
            P.op("dve", lambda e, g=g, dst=dst: e.tensor_tensor(
                dst[:, :, :], L_all[:, :, 4 + 8 * g:12 + 8 * g],
                ohg[:, :, g:g + 1].to_broadcast([128, NT, 8]), ALU.mult), r=LN_ + ["ohg"], w=[dn])
            if g > 0:
                P.op("dve", lambda e: e.tensor_tensor(el[:, :, :], el[:, :, :], elt[:, :, :], ALU.add),
                     r=["el", "elt"], w=["el"])
        P.op("dve", lambda e: e.reduce_max(m1[:, :], el[:, :, :], AX.X), r=["el"], w=["m1"])
        P.op("dve", lambda e: e.tensor_tensor(oh1[:, :, :], el[:, :, :], bc(m1, 8), ALU.is_equal),
             r=["el", "m1"], w=["oh1"])
        P.op("dve", lambda e: e.scalar_tensor_tensor(
            el2[:, :, :].rearrange("p a b -> p (a b)"), oh1[:, :, :].rearrange("p a b -> p (a b)"), -1e30,
            el[:, :, :].rearrange("p a b -> p (a b)"), ALU.mult, ALU.add), r=["oh1", "el"], w=["el2"])
        P.op("dve", lambda e: e.reduce_max(m2[:, :], el2[:, :, :], AX.X), r=["el2"], w=["m2"])
        P.op("dve", lambda e: e.tensor_tensor(oh2[:, :, :], el2[:, :, :], bc(m2, 8), ALU.is_equal),
             r=["el2", "m2"], w=["oh2"])
        P.op("dve", lambda e: e.tensor_tensor(dd[:, :], m2[:, :], m1[:, :], ALU.subtract), r=["m1", "m2"], w=["dd"])
        P.op("act", lambda e: e.activation(dd[:, :], dd[:, :], AF.Exp), r=["dd"], w=["dd"])
        w1 = w12[:, 0, :]
        w2 = w12[:, 1, :]
        P.op("dve", lambda e: e.tensor_scalar(w1, dd[:, :], 1.0, None, ALU.add), r=["dd"], w=["w1"])
        P.op("dve", lambda e: e.reciprocal(w1, w1), r=["w1"], w=["w1"])
        P.op("dve", lambda e: e.tensor_tensor(w1, w1, gw[:, :], ALU.mult), r=["w1", "gw"], w=["w1"])
        P.op("dve", lambda e: e.tensor_tensor(w2, dd[:, :], w1, ALU.mult), r=["dd", "w1"], w=["w2"])
        for g in range(4):
            P.op("dve", lambda e, g=g: e.tensor_tensor(
                A1[:, :, 8 * g:8 * g + 8], oh1[:, :, :], ohg[:, :, g:g + 1].to_broadcast([128, NT, 8]), ALU.mult),
                r=["oh1", "ohg"], w=[f"A1_{g}"])
            P.op("dve", lambda e, g=g: e.tensor_tensor(
                A2[:, :, 8 * g:8 * g + 8], oh2[:, :, :], ohg[:, :, g:g + 1].to_broadcast([128, NT, 8]), ALU.mult),
                r=["oh2", "ohg"], w=[f"A2_{g}"])
        A1N = [f"A1_{g}" for g in range(4)]
        A2N = [f"A2_{g}" for g in range(4)]
        P.op("dve", lambda e: e.tensor_tensor(Ab[:, :, :], A1[:, :, :], A2[:, :, :], ALU.add), r=A1N + A2N, w=["Ab"])
        bin_, bi_ = bank()
        bcn_, bc_ = bank()
        Abf = Ab[:, :, :].rearrange("p a b -> p (a b)")
        P.op("pe", lambda e: e.matmul(bi_[:, :], tri[:, :], Abf, start=True, stop=True), r=["tri", "Ab"], w=[bin_])
        P.op("pe", lambda e: e.matmul(bc_[:, :], ones[:, :], Abf, start=True, stop=True), r=["ones", "Ab"], w=[bcn_])
        cnt3 = bc_[:, :].rearrange("p (a b) -> p a b", a=NT)
        P.op("dve", lambda e: e.memset(pre[:, 0, :], 0.0), w=["pre"])
        for i in range(NT):
            P.op("dve", lambda e, i=i: e.tensor_tensor(pre[:, i + 1, :], pre[:, i, :], cnt3[:, i, :], ALU.add),
                 r=["pre", bcn_], w=["pre"])
        tot = pre[:, NT, :]
        thr = cst[:, 0:16]
        sidx = cst[:, 16:80]
        P.op("dve", lambda e: e.tensor_tensor(cmpa[:, :, :], tot.unsqueeze(2).to_broadcast([128, 32, 16]),
                                              thr.unsqueeze(1).to_broadcast([128, 32, 16]), ALU.is_gt),
             r=["pre", "cst"], w=["cmpa"])
        P.op("dve", lambda e: e.reduce_sum(ns[:, :], cmpa[:, :, :], AX.X), r=["cmpa"], w=["ns"])
        cur, cur_n, oth, oth_n = ns, "ns", scA, "scA"
        for d in (1, 2, 4, 8, 16):
            P.op("dve", lambda e, d=d, cur=cur, oth=oth: e.tensor_tensor(oth[:, d:32], cur[:, d:32], cur[:, 0:32 - d],
                                                                         ALU.add), r=[cur_n], w=[oth_n + "t"])
            P.op("dve", lambda e, d=d, cur=cur, oth=oth: e.tensor_copy(oth[:, 0:d], cur[:, 0:d]),
                 r=[cur_n], w=[oth_n + "h"])
            nxt, nxt_n = (scB, "scB") if oth is scA else (scA, "scA")
            cur, cur_n, oth, oth_n = oth, oth_n, nxt, nxt_n
            cur_n_list = [cur_n + "t", cur_n + "h"]
            P.lastw[cur_n] = max((P.lastw[cur_n + "t"], P.lastw[cur_n + "h"]), key=lambda t: t[1])
            P.readers[cur_n] = []
        incl, incl_n = cur, cur_n
        P.op("dve", lambda e: e.tensor_tensor(sbase[:, :], incl[:, :], ns[:, :], ALU.subtract),
             r=[incl_n, "ns"], w=["sbase"])
        P.op("dve", lambda e: e.tensor_tensor(cmpb[:, :, :], incl[:, :].unsqueeze(1).to_broadcast([128, 64, 32]),
                                              sidx.unsqueeze(2).to_broadcast([128, 64, 32]), ALU.is_le),
             r=[incl_n, "cst"], w=["cmpb"])
        P.op("dve", lambda e: e.reduce_sum(esf[:, :], cmpb[:, :, :], AX.X), r=["cmpb"], w=["esf"])
        P.op("dve", lambda e: e.tensor_scalar(esf[:, :], esf[:, :], 128.0, cst[:, 80:81], ALU.mult, ALU.add),
             r=["esf", "cst"], w=["esf"])
        P.op("dve", lambda e: e.tensor_copy(widx[:, :], esf[:, :]), r=["esf"], w=["widx"])
        P.op("dve", lambda e: e.tensor_tensor(R[:, :, :], bi_[:, :].rearrange("p (a b) -> p a b", a=NT),
                                              pre[:, 0:NT, :], ALU.add), r=[bin_, "pre"], w=["R"])
        P.op("dve", lambda e: e.scalar_tensor_tensor(
            R[:, :, :], sbase[:, :].unsqueeze(1).to_broadcast([128, NT, 32]), float(CAP), R[:, :, :], ALU.mult, ALU.add),
            r=["sbase", "R"], w=["R"])
        for k_, (Ak, AkN) in enumerate(((A1, A1N), (A2, A2N))):
            P.op("dve", lambda e, Ak=Ak: e.tensor_tensor(prod[:, :, :], Ak[:, :, :], R[:, :, :], ALU.mult),
                 r=AkN + ["R"], w=["prod"])
            P.op("dve", lambda e, k_=k_: e.reduce_sum(posf[:, :, k_], prod[:, :, :], AX.X), r=["prod"], w=[f"posf{k_}"])
        P.op("dve", lambda e: e.tensor_copy(pos_i[:, :], posf[:, :, :].rearrange("p a b -> p (a b)")),
             r=["posf0", "posf1"], w=["pos_i"])

        xsb = [sb(f"xsb{i}", [128, D], BF16) for i in range(3)]
        xT = [sb(f"xT{i}", [128, 8, 128], BF16) for i in range(2)]
        wgu = [sb(f"wgu{i}", [128, 8, 512], BF16) for i in range(3)]
        wdn = [sb(f"wdn{i}", [128, 2, 1024], BF16) for i in range(3)]
        sg = [sb(f"sg{i}", [128, DE], F32) for i in range(2)]
        actb = [sb(f"actb{i}", [128, DE], BF16) for i in range(2)]
        actT = [sb(f"actT{i}", [128, 2, 128], BF16) for i in range(2)]
        ysb = [sb(f"ysb{i}", [128, D], BF16) for i in range(2)]
        y0 = [sb(f"y0_{i}", [128, D], BF16) for i in range(4)]
        y1 = [sb(f"y1_{i}", [128, D], BF16) for i in range(4)]
        bt = [sb(f"bt{i}", [128, D], F32) for i in range(4)]
        ot = [sb(f"ot{i}", [128, D], F32) for i in range(4)]
        tmpB = sb("tmpB", [128, D], F32)
        vbc2 = sb("vbc2", [128, 2, D], F32)
        for j in (5, 6):
            P.dma("sp", lambda e, j=j: e.dma_start(
                out=vbc2[:, j - 5, :], in_=dram_ap(vecs_d, j * D, [[0, 128], [1, D]])), w=[f"vbc2_{j - 5}"])

        breg = {}

        def wfetch(S):
            wi = S % 3
            for nm, dst, src in ((f"wgu{wi}_0", wgu[wi][:, 0:4, :].rearrange("p k f -> p (k f)"), wgu0_d),
                                 (f"wgu{wi}_1", wgu[wi][:, 4:8, :].rearrange("p k f -> p (k f)"), wgu1_d),
                                 (f"wdn{wi}", wdn[wi][:, :, :].rearrange("p k f -> p (k f)"), wdn_d)):
                def fetch(e, dst=dst, src=src):
                    if "b" not in breg:
                        breg["b"] = e.alloc_register("wbound")
                        e.reg_mov(breg["b"], NEXP * 128 - 1)
                    return e.indirect_dma_start(
                        out=dst, out_offset=None, in_=src[:, :, :].rearrange("e p f -> (e p) f"),
                        in_offset=bass.IndirectOffsetOnAxis(ap=widx[:, S:S + 1], axis=0),
                        bounds_check=breg["b"], oob_is_err=False)
                P.dma("pool", fetch, r=["widx"], w=[nm])

        def xload(hs_):
            xi = hs_ % 3
            P.dma("sp", lambda e: e.dma_start(out=xsb[xi][:, :], in_=xs_d[hs_ * 128:(hs_ + 1) * 128, :]),
                  r=XS, w=[f"xsb{xi}"])

        GUB = [("psF0", psF[:, 0:512]), ("psF1", psF[:, 512:1024])]
        YP = [(("psF2", "psF3"), psF[:, 1024:2048]), (("psF4", "psF5"), psF[:, 2048:3072])]

        def slot_s1(hs_):
            S = hs_ // HPS
            wi = S % 3
            si = hs_ % 2
            if hs_ + 2 < NSLOT * HPS:
                xload(hs_ + 2)
            transposes(xsb[hs_ % 3], f"xsb{hs_ % 3}", si, 128, 8)
            P.op("act", lambda e: e.copy(xT[si][:, :, :], psT[si][:, :].rearrange("p (k t) -> p k t", k=8)),
                 r=[f"psT{si}"], w=[f"xT{si}"])

        def slot_s1b(hs_):
            S = hs_ // HPS
            wi = S % 3
            si = hs_ % 2
            bn, b = GUB[si]
            P.op("pe", [lambda e, k=k: e.matmul(b[:, :], xT[si][:, k, :], wgu[wi][:, k, :],
                                                start=(k == 0), stop=(k == 7)) for k in range(8)],
                 r=[f"xT{si}", f"wgu{wi}_0", f"wgu{wi}_1"], w=[bn])
            P.op("act", lambda e: e.activation(sg[si][:, :], b[:, 0:DE], AF.Silu), r=[bn], w=[f"sg{si}"])
            P.op("dve", lambda e: e.tensor_tensor(actb[si][:, :], b[:, DE:2 * DE], sg[si][:, :], ALU.mult),
                 r=[bn, f"sg{si}"], w=[f"actb{si}"])

        def slot_s2(hs_):
            S = hs_ // HPS
            wi = S % 3
            si = hs_ % 2
            ps = psT[si]
            P.op("pe", [lambda e, c=c: e.transpose(ps[:, c * 128:(c + 1) * 128], actb[si][:, c * 128:(c + 1) * 128],
                                                   ident[:, :]) for c in range(2)],
                 r=[f"actb{si}", "ident"], w=[f"psT{si}"])
            P.op("act", lambda e: e.copy(actT[si][:, :, :].rearrange("p c t -> p (c t)"), ps[:, 0:256]),
                 r=[f"psT{si}"], w=[f"actT{si}"])
            (yn0, yn1), py = YP[si]
            fns = []
            for hf in range(2):
                for c in range(2):
                    fns.append(lambda e, hf=hf, c=c: e.matmul(
                        py[:, hf * 512:(hf + 1) * 512], actT[si][:, c, :], wdn[wi][:, c, hf * 512:(hf + 1) * 512],
                        start=(c == 0), stop=(c == 1)))
            P.op("pe", fns, r=[f"actT{si}", f"wdn{wi}"], w=[yn0, yn1])
            P.op("dve", lambda e: e.tensor_copy(ysb[si][:, :], py[:, :]), r=[yn0, yn1], w=[f"ysb{si}"])
            P.dma("sp", lambda e: e.dma_start(out=ys_d[hs_ * 128:(hs_ + 1) * 128, :], in_=ysb[si][:, :]),
                  r=[f"ysb{si}"], w=[f"ys_d{hs_}"], chan=f"yst{si}")

        wfetch(0)
        for i in range(NT):
            for k_ in range(2):
                c = 2 * i + k_
                P.dma("pool", lambda e, i=i, c=c: e.indirect_dma_start(
                    out=xs_d[:, :], out_offset=bass.IndirectOffsetOnAxis(ap=pos_i[:, c:c + 1], axis=0),
                    in_=h1b_all[:, i, :], in_offset=None), r=[f"h1b{i}", "pos_i"] + XZ, w=[f"xs_s{c}"],
                    chan=f"sc{c % 4}")
        XS = [f"xs_s{c}" for c in range(2 * NT)]
        wfetch(1)
        wfetch(2)

        xload(0)
        xload(1)
        NH = NSLOT * HPS
        slot_s1(0)
        slot_s1(1)
        slot_s1b(0)
        for hs_ in range(NH):
            if hs_ + 2 < NH:
                slot_s1(hs_ + 2)
            if hs_ + 1 < NH:
                slot_s1b(hs_ + 1)
            slot_s2(hs_)
            if hs_ % HPS == HPS - 1 and hs_ // HPS + 3 < NSLOT:
                wfetch(hs_ // HPS + 3)
        YS = [f"ys_d{hs_}" for hs_ in range(NSLOT * HPS)]

        def c_prefetch(i):
            oi = i % 4
            P.dma("sp", lambda e: e.dma_start(out=bt[oi][:, :], in_=base_d[i * 128:(i + 1) * 128, :]),
                  r=[f"base_d{i}"], w=[f"bt{oi}"])
            for k_, (yy, yn) in enumerate(((y0[oi], f"y0_{oi}"), (y1[oi], f"y1_{oi}"))):
                c = 2 * i + k_
                P.dma("pool", lambda e, yy=yy, c=c: e.indirect_dma_start(
                    out=yy[:, :], out_offset=None, in_=ys_d[:, :],
                    in_offset=bass.IndirectOffsetOnAxis(ap=pos_i[:, c:c + 1], axis=0)),
                    r=YS + ["pos_i"], w=[yn])

        junk = [sb(f"junk{i}", [128, D], F32) for i in range(2)]
        st2 = [sb(f"st2_{i}", [128, 8], F32) for i in range(4)]

        def layer_norm_act(src, src_n, tmp, tmp_n, dst, dst_n, j):
            st = st2[j]
            sn = f"st2_{j}"
            P.op("act", lambda e: e.activation(junk[0][:, :], src[:, :], AF.Identity, accum_out=st[:, 0:1]),
                 r=[src_n], w=["junk0", sn + "s1"])
            P.op("act", lambda e: e.activation(junk[1][:, :], src[:, :], AF.Square, accum_out=st[:, 1:2]),
                 r=[src_n], w=["junk1", sn + "s2"])
            P.op("pool", lambda e: e.tensor_scalar(st[:, 2:3], st[:, 0:1], 1.0 / D, None, ALU.mult),
                 r=[sn + "s1"], w=[sn + "mean"])
            P.op("pool", lambda e: e.tensor_tensor(st[:, 3:4], st[:, 2:3], st[:, 2:3], ALU.mult),
                 r=[sn + "mean"], w=[sn + "msq"])
            P.op("pool", lambda e: e.tensor_scalar(st[:, 4:5], st[:, 1:2], 1.0 / D, LN_EPS, ALU.mult, ALU.add),
                 r=[sn + "s2"], w=[sn + "ve"])
            P.op("pool", lambda e: e.tensor_tensor(st[:, 4:5], st[:, 4:5], st[:, 3:4], ALU.subtract),
                 r=[sn + "ve", sn + "msq"], w=[sn + "ve"])
            P.op("pool", lambda e: e.tensor_tensor(st[:, 5:6], st[:, 4:5], neghalf[:, :], ALU.pow),
                 r=[sn + "ve", "neghalf"], w=[sn + "rs"])
            P.op("dve", lambda e: e.scalar_tensor_tensor(tmp[:, :], src[:, :], st[:, 2:3], vbc2[:, 0, :],
                                                         ALU.subtract, ALU.mult),
                 r=[src_n, sn + "mean", "vbc2_0"], w=[tmp_n])
            P.op("dve", lambda e: e.scalar_tensor_tensor(dst[:, :], tmp[:, :], st[:, 5:6], vbc2[:, 1, :],
                                                         ALU.mult, ALU.add),
                 r=[tmp_n, sn + "rs", "vbc2_1"], w=[dst_n])

        def final_tile(i):
            oi = i % 4
            P.op("dve", lambda e: e.scalar_tensor_tensor(bt[oi][:, :], y0[oi][:, :], w12[:, 0, i:i + 1], bt[oi][:, :],
                                                         ALU.mult, ALU.add), r=[f"y0_{oi}", "w1", f"bt{oi}"], w=[f"bt{oi}"])
            P.op("dve", lambda e: e.scalar_tensor_tensor(ot[oi][:, :], y1[oi][:, :], w12[:, 1, i:i + 1], bt[oi][:, :],
                                                         ALU.mult, ALU.add), r=[f"y1_{oi}", "w2", f"bt{oi}"], w=[f"ot{oi}"])
            if i + 3 < NT:
                c_prefetch(i + 3)
            layer_norm_act(ot[oi], f"ot{oi}", tmpB, "tmpB", ot[oi], f"ot{oi}", oi)
            P.dma("sp", lambda e: e.dma_start(out=out_d[i * 128:(i + 1) * 128, :], in_=ot[oi][:, :]),
                  r=[f"ot{oi}"], chan=f"ost{oi}")

        c_prefetch(0)
        c_prefetch(1)
        c_prefetch(2)
        for i in range(NT):
            final_tile(i)

    P.final_wait("sp")
    _DBG["P"] = P

    sems = {}
    for sk in P.count:
        sems[sk] = nc.alloc_semaphore(sk)
    with nc.Block() as block:
        @block.tensor
        def _(e):
            P.emit("pe", e, sems)

        @block.scalar
        def _(e):
            P.emit("act", e, sems)

        @block.vector
        def _(e):
            P.emit("dve", e, sems)

        @block.gpsimd
        def _(e):
            P.emit("pool", e, sems)

        @block.sync
        def _(e):
            P.emit("sp", e, sems)
    return nc


def _prep_inputs(x, p, ln_in_g, ln_in_b, w_in, conv_w, conv_b, pool_w, pool_scale, w_out, ln1_g, ln1_b,
                 w_rg, b_rg, w_re, b_re, w_gate, w_up, w_down, w_pg, b_pg, w_ple, ln2_g, ln2_b):
    f = np.float32
    x = np.asarray(x, f)
    p = np.asarray(p, f)[0]
    B, S, _ = x.shape
    vecs = np.stack([np.asarray(v, f).reshape(-1) for v in
                     (ln_in_g, ln_in_b, ln1_g, ln1_b, b_pg, ln2_g, ln2_b)], axis=0)
    wr = np.concatenate([np.asarray(w_rg, f)[0]] + [np.asarray(w_re, f)[0, g] for g in range(4)], axis=1)
    br = np.concatenate([np.asarray(b_rg, f)[0]] + [np.asarray(b_re, f)[0, g] for g in range(4)], axis=0)
    poolw = np.ascontiguousarray(np.asarray(pool_w, f)[0].transpose(1, 0, 2)).reshape(128, 512)
    cw = np.asarray(conv_w, f)[0].reshape(3, 4, 128)
    chv = np.concatenate([
        cw.transpose(2, 1, 0).reshape(128, 12),
        np.asarray(conv_b, f)[0].reshape(4, 128).T,
        np.asarray(pool_scale, f)[0].reshape(4, 128).T], axis=1)
    wg = np.asarray(w_gate, f)[0].reshape(NEXP, 8, 128, DE)
    wu = np.asarray(w_up, f)[0].reshape(NEXP, 8, 128, DE)
    wgu = np.concatenate([wg, wu], axis=3).transpose(0, 2, 1, 3).reshape(NEXP, 128, 8 * 512)
    wgu = np.ascontiguousarray(wgu)
    wdn = np.ascontiguousarray(
        np.asarray(w_down, f)[0].reshape(NEXP, 2, 128, D).transpose(0, 2, 1, 3)).reshape(NEXP, 128, 2 * D)
    shared = dict(
        ident=np.eye(128, dtype=f), vecs=np.ascontiguousarray(vecs), br=np.ascontiguousarray(br),
        w_in=np.ascontiguousarray(np.asarray(w_in, f)[0]), w_out=np.ascontiguousarray(np.asarray(w_out, f)[0]),
        w_pg=np.ascontiguousarray(np.asarray(w_pg, f)[0]), w_ple=np.ascontiguousarray(np.asarray(w_ple, f)[0]),
        wr=np.ascontiguousarray(wr), poolw=poolw, chv=np.ascontiguousarray(chv), wgu0=np.ascontiguousarray(wgu[:, :, 0:2048]),
        wgu1=np.ascontiguousarray(wgu[:, :, 2048:4096]), wdn=wdn,
        tri=np.triu(np.ones((128, 128), f), 1),
        zsrc=np.zeros((1024, D), ml_dtypes.bfloat16),
        cst=np.ascontiguousarray(np.concatenate([np.broadcast_to(np.concatenate(
            [np.arange(16, dtype=f) * float(CAP), np.arange(64, dtype=f)])[None, :], (128, 80)),
            np.arange(128, dtype=f)[:, None],
            (np.arange(NSLOT * HPS, dtype=f)[None, :] * 128.0 + np.arange(128, dtype=f)[:, None]),
            np.broadcast_to(np.array([0.0, -128.0], f)[None, :], (128, 2))], axis=1)))
    in_maps = []
    for c in range(NCORES):
        b, half = c // 2, c % 2
        s0 = half * TOK
        xl = np.zeros((TOK + HALO, D), f)
        if half == 0:
            xl[HALO:] = x[b, 0:TOK]
            hm = np.zeros((128, HALO), f)
            ic = np.stack([1.0 / np.minimum(np.arange(HALO) + 1, w) for w in POOL_W]).astype(f)
        else:
            xl[:] = x[b, s0 - HALO:s0 + TOK]
            hm = np.ones((128, HALO), f)
            ic = np.stack([np.full(HALO, 1.0 / w) for w in POOL_W]).astype(f)
        icb = np.ascontiguousarray(np.broadcast_to(ic.reshape(1, 4 * HALO), (128, 4 * HALO))).astype(f)
        m = dict(shared)
        m.update(x=xl, p=np.ascontiguousarray(p[b, s0:s0 + TOK]), hmask=hm, invcnt=icb)
        in_maps.append(m)
    return in_maps, (B, S)


_NC_CACHE = {}


def kernel(**inputs):
    in_maps, (B, S) = _prep_inputs(**inputs)
    if "nc" not in _NC_CACHE:
        _NC_CACHE["nc"] = build_nc()
    nc = _NC_CACHE["nc"]
    res = run_bass_kernel_spmd(nc, in_maps, core_ids=list(range(NCORES)))
    out = np.empty((B, S, D), np.float32)
    for c in range(NCORES):
        b, half = c // 2, c % 2
        out[b, half * TOK:(half + 1) * TOK] = res.results[c]["out"]
    return out
```
